# Optimizing a Trainium2 kernel written in Bass

```python
import math
import jax, jax.numpy as jnp
from jax import lax
import numpy as np

D_MODEL = 2048
BATCH = 2
SEQ = 4096
DEPTH = 2

HEAD_DIM = 128
N_HEADS = D_MODEL // HEAD_DIM
NA_HEADS = N_HEADS // 2
DA_HEADS = N_HEADS - NA_HEADS
DA_QK_DIM = HEAD_DIM // 2
GRID_W = 64
NA_KH = 8
NA_KW = 16
ROPE_THETA = 10000.0
Q_BLOCK = 128
IN_COLS = 3 * (NA_HEADS + DA_HEADS) * HEAD_DIM
MIX_WIDTH = (NA_HEADS + DA_HEADS) * HEAD_DIM
LRU_WIDTH = D_MODEL
LRU_BLOCKS = 16
LRU_BLOCK_DIM = LRU_WIDTH // LRU_BLOCKS
CONV_WIDTH = 4
CONV_PAD_LEFT = (CONV_WIDTH - 1) // 2
CONV_PAD_RIGHT = CONV_WIDTH - 1 - CONV_PAD_LEFT
LRU_C = 8.0
D_FF = 7 * D_MODEL // 2
N_EXPERTS = 8
TOP_K = 2
D_FF_EXPERT = 7 * D_MODEL // 2
N_EVEN = (DEPTH + 1) // 2
N_ODD = DEPTH // 2
EPS = 1e-6
NEG_INF = -1e30

kernel_name = "hybrid_natten_diffattn_rglru_moe_encoder"


def rms_norm(x, g):
    xf = x.astype(jnp.float32)
    y = xf * lax.rsqrt(jnp.mean(xf * xf, axis=-1, keepdims=True) + EPS)
    return (y * g.astype(jnp.float32)).astype(x.dtype)


def swiglu(h, wg, wu, wd):
    return (jax.nn.silu(h @ wg) * (h @ wu)) @ wd


def rope_tables(seq, dim):
    inv = 1.0 / (ROPE_THETA ** (jnp.arange(0, dim, 2, dtype=jnp.float32) / dim))
    ang = jnp.arange(seq, dtype=jnp.float32)[:, None] * inv[None, :]
    return jnp.cos(ang), jnp.sin(ang)


def apply_rope(x, cos, sin):
    x1, x2 = jnp.split(x, 2, axis=-1)
    c, s = cos.astype(x.dtype), sin.astype(x.dtype)
    return jnp.concatenate([x1 * c - x2 * s, x2 * c + x1 * s], axis=-1)


def neighbourhood_attention(q, k, v, rpb):
    b, s, h, d = q.shape
    rows = s // GRID_W
    kh = min(NA_KH, rows)
    kw = min(NA_KW, GRID_W)
    r = jnp.arange(rows)
    r_start = jnp.clip(r - kh // 2, 0, rows - kh)
    row_idx = r_start[:, None] + jnp.arange(kh)[None, :]
    row_off = row_idx - r[:, None]
    c = jnp.arange(GRID_W)
    c_start = jnp.clip(c - kw // 2, 0, GRID_W - kw)
    col_in = (c[None, :] >= c_start[:, None]) & (c[None, :] < c_start[:, None] + kw)
    col_off = jnp.clip(c[None, :] - c[:, None], -(NA_KW - 1), NA_KW - 1)
    qg = q.reshape(b, rows, GRID_W, h, d)
    kg = k.reshape(b, rows, GRID_W, h, d)[:, row_idx]
    vg = v.reshape(b, rows, GRID_W, h, d)[:, row_idx]
    scores = jnp.einsum('brqhd,brikhd->bhrqik', qg, kg,
                        preferred_element_type=jnp.float32) * (d ** -0.5)
    bias = rpb[:, (row_off + NA_KH - 1)[:, None, :, None],
               (col_off + NA_KW - 1)[None, :, None, :]].astype(jnp.float32)
    scores = jnp.where(col_in[:, None, :], scores + bias[None], NEG_INF)
    p = jax.nn.softmax(scores.reshape(b, h, rows, GRID_W, kh * GRID_W), axis=-1)
    p = p.reshape(b, h, rows, GRID_W, kh, GRID_W).astype(v.dtype)
    out = jnp.einsum('bhrqik,brikhd->brqhd', p, vg)
    return out.reshape(b, s, h * d)


def differential_attention(q, k, v, lq1, lk1, lq2, lk2, subln, lambda_init):
    b, s, h, _, dq = q.shape
    dv = v.shape[-1]
    cos, sin = rope_tables(s, dq)
    cos, sin = cos[:, None, None, :], sin[:, None, None, :]
    q = apply_rope(q, cos, sin)
    k = apply_rope(k, cos, sin)
    lam = (jnp.exp(jnp.sum((lq1 * lk1).astype(jnp.float32)))
           - jnp.exp(jnp.sum((lq2 * lk2).astype(jnp.float32))) + lambda_init)
    scale = dq ** -0.5
    n_blk = s // Q_BLOCK
    qb = q.reshape(b, n_blk, Q_BLOCK, h, 2, dq).transpose(1, 0, 2, 3, 4, 5)

    def block(q_blk):
        sc = jnp.einsum('bqhcd,bkhcd->bchqk', q_blk, k,
                        preferred_element_type=jnp.float32) * scale
        p = jax.nn.softmax(sc, axis=-1)
        attn = (p[:, 0] - lam * p[:, 1]).astype(v.dtype)
        return jnp.einsum('bhqk,bkhd->bqhd', attn, v)

    o = lax.map(block, qb)
    o = o.transpose(1, 0, 2, 3, 4).reshape(b, s, h, dv)
    o = rms_norm(o, subln) * (1.0 - lambda_init)
    return o.reshape(b, s, h * dv)


def _lin_combine(left, right):
    a1, b1 = left
    a2, b2 = right
    return a1 * a2, a2 * b1 + b2


def rg_lru(u, w_a, b_a, w_x, b_x, a_param, reverse):
    b, s, _ = u.shape
    ub = u.reshape(b, s, LRU_BLOCKS, LRU_BLOCK_DIM)
    gate_a = jax.nn.sigmoid(jnp.einsum('bsni,nij->bsnj', ub, w_a) + b_a.reshape(LRU_BLOCKS, LRU_BLOCK_DIM))
    gate_x = jax.nn.sigmoid(jnp.einsum('bsni,nij->bsnj', ub, w_x) + b_x.reshape(LRU_BLOCKS, LRU_BLOCK_DIM))
    gate_a = gate_a.reshape(b, s, LRU_WIDTH).astype(jnp.float32)
    gate_x = gate_x.reshape(b, s, LRU_WIDTH)
    log_a = -LRU_C * gate_a * jax.nn.softplus(-a_param.astype(jnp.float32))
    a = jnp.exp(log_a)
    mult = jnp.sqrt(-jnp.expm1(2.0 * log_a))
    bt = mult * (gate_x * u).astype(jnp.float32)
    _, hs = lax.associative_scan(_lin_combine, (a, bt), axis=1, reverse=reverse)
    return hs.astype(u.dtype)


def moe_swiglu(h, router, wg, wu, wd):
    b, s, d = h.shape
    t = h.reshape(b * s, d)
    logits = (t @ router).astype(jnp.float32)
    top_vals, top_idx = lax.top_k(logits, TOP_K)
    gates = jax.nn.softmax(top_vals, axis=-1)
    combine = jnp.sum(jax.nn.one_hot(top_idx, N_EXPERTS, dtype=jnp.float32) * gates[..., None], axis=1)
    combine = combine.astype(t.dtype)
    out = jnp.zeros_like(t)
    for e in range(N_EXPERTS):
        out = out + combine[:, e:e + 1] * swiglu(t, wg[e], wu[e], wd[e])
    return out.reshape(b, s, d)


def setup_inputs(seed: int = 0) -> dict:
    key = jax.random.key(seed)
    ks = iter(jax.random.split(key, 40))
    nrm = lambda shape, scale: jax.random.normal(next(ks), shape, jnp.float32) * scale
    gain = lambda shape: 1.0 + nrm(shape, 0.01)
    NE, NO, D = N_EVEN, N_ODD, D_MODEL
    a_init = jax.random.uniform(next(ks), (NO, 2, LRU_WIDTH), jnp.float32, 0.9, 0.999)
    return {
        "x": nrm((BATCH, SEQ, D), 1.0),
        "ev_mix_norm": gain((NE, D)),
        "ev_w_in": nrm((NE, D, IN_COLS), D ** -0.5),
        "ev_na_rpb": nrm((NE, NA_HEADS, 2 * NA_KH - 1, 2 * NA_KW - 1), 0.1),
        "ev_da_lambda_q1": nrm((NE, DA_QK_DIM), 0.1),
        "ev_da_lambda_k1": nrm((NE, DA_QK_DIM), 0.1),
        "ev_da_lambda_q2": nrm((NE, DA_QK_DIM), 0.1),
        "ev_da_lambda_k2": nrm((NE, DA_QK_DIM), 0.1),
        "ev_da_subln": gain((NE, HEAD_DIM)),
        "ev_w_out": nrm((NE, MIX_WIDTH, D), MIX_WIDTH ** -0.5),
        "ev_ffn_norm": gain((NE, D)),
        "ev_ffn_w_gate": nrm((NE, D, D_FF), D ** -0.5),
        "ev_ffn_w_up": nrm((NE, D, D_FF), D ** -0.5),
        "ev_ffn_w_down": nrm((NE, D_FF, D), D_FF ** -0.5),
        "od_mix_norm": gain((NO, D)),
        "od_w_in": nrm((NO, D, 2 * LRU_WIDTH), D ** -0.5),
        "od_conv_w": nrm((NO, CONV_WIDTH, LRU_WIDTH), CONV_WIDTH ** -0.5),
        "od_conv_b": nrm((NO, LRU_WIDTH), 0.01),
        "od_lru_w_a": nrm((NO, 2, LRU_BLOCKS, LRU_BLOCK_DIM, LRU_BLOCK_DIM), LRU_BLOCK_DIM ** -0.5),
        "od_lru_b_a": nrm((NO, 2, LRU_WIDTH), 0.1),
        "od_lru_w_x": nrm((NO, 2, LRU_BLOCKS, LRU_BLOCK_DIM, LRU_BLOCK_DIM), LRU_BLOCK_DIM ** -0.5),
        "od_lru_b_x": nrm((NO, 2, LRU_WIDTH), 0.1),
        "od_lru_a_param": jnp.log(a_init) - jnp.log1p(-a_init),
        "od_w_out": nrm((NO, LRU_WIDTH, D), LRU_WIDTH ** -0.5),
        "od_ffn_norm": gain((NO, D)),
        "od_router": nrm((NO, D, N_EXPERTS), D ** -0.5),
        "od_moe_w_gate": nrm((NO, N_EXPERTS, D, D_FF_EXPERT), D ** -0.5),
        "od_moe_w_up": nrm((NO, N_EXPERTS, D, D_FF_EXPERT), D ** -0.5),
        "od_moe_w_down": nrm((NO, N_EXPERTS, D_FF_EXPERT, D), D_FF_EXPERT ** -0.5),
        "final_norm": gain((D,)),
    }


def reference(x, ev_mix_norm, ev_w_in, ev_na_rpb, ev_da_lambda_q1, ev_da_lambda_k1,
              ev_da_lambda_q2, ev_da_lambda_k2, ev_da_subln, ev_w_out, ev_ffn_norm,
              ev_ffn_w_gate, ev_ffn_w_up, ev_ffn_w_down, od_mix_norm, od_w_in, od_conv_w,
              od_conv_b, od_lru_w_a, od_lru_b_a, od_lru_w_x, od_lru_b_x, od_lru_a_param,
              od_w_out, od_ffn_norm, od_router, od_moe_w_gate, od_moe_w_up, od_moe_w_down,
              final_norm):
    b, s, _ = x.shape
    na_w = NA_HEADS * HEAD_DIM
    da_w = DA_HEADS * HEAD_DIM
    split_at = (na_w, 2 * na_w, 3 * na_w, 3 * na_w + da_w, 3 * na_w + 2 * da_w)
    for layer in range(DEPTH):
        if layer % 2 == 0:
            i = layer // 2
            lambda_init = 0.8 - 0.6 * math.exp(-0.3 * layer)
            h = rms_norm(x, ev_mix_norm[i])
            proj = h @ ev_w_in[i]
            na_q, na_k, na_v, da_q, da_k, da_v = jnp.split(proj, split_at, axis=-1)
            na_out = neighbourhood_attention(
                na_q.reshape(b, s, NA_HEADS, HEAD_DIM), na_k.reshape(b, s, NA_HEADS, HEAD_DIM),
                na_v.reshape(b, s, NA_HEADS, HEAD_DIM), ev_na_rpb[i])
            da_out = differential_attention(
                da_q.reshape(b, s, DA_HEADS, 2, DA_QK_DIM), da_k.reshape(b, s, DA_HEADS, 2, DA_QK_DIM),
                da_v.reshape(b, s, DA_HEADS, HEAD_DIM), ev_da_lambda_q1[i], ev_da_lambda_k1[i],
                ev_da_lambda_q2[i], ev_da_lambda_k2[i], ev_da_subln[i], lambda_init)
            x = x + jnp.concatenate([na_out, da_out], axis=-1) @ ev_w_out[i]
            h = rms_norm(x, ev_ffn_norm[i])
            x = x + swiglu(h, ev_ffn_w_gate[i], ev_ffn_w_up[i], ev_ffn_w_down[i])
        else:
            i = layer // 2
            h = rms_norm(x, od_mix_norm[i])
            proj = h @ od_w_in[i]
            gate_branch, rec_branch = jnp.split(proj, 2, axis=-1)
            y = jax.nn.gelu(gate_branch)
            u = lax.conv_general_dilated(
                rec_branch, od_conv_w[i][:, None, :], window_strides=(1,),
                padding=[(CONV_PAD_LEFT, CONV_PAD_RIGHT)],
                dimension_numbers=('NWC', 'WIO', 'NWC'),
                feature_group_count=LRU_WIDTH) + od_conv_b[i]
            h_fwd = rg_lru(u, od_lru_w_a[i, 0], od_lru_b_a[i, 0], od_lru_w_x[i, 0],
                           od_lru_b_x[i, 0], od_lru_a_param[i, 0], reverse=False)
            h_bwd = rg_lru(u, od_lru_w_a[i, 1], od_lru_b_a[i, 1], od_lru_w_x[i, 1],
                           od_lru_b_x[i, 1], od_lru_a_param[i, 1], reverse=True)
            x = x + (y * (h_fwd + h_bwd)) @ od_w_out[i]
            h = rms_norm(x, od_ffn_norm[i])
            x = x + moe_swiglu(h, od_router[i], od_moe_w_gate[i], od_moe_w_up[i], od_moe_w_down[i])
    return rms_norm(x, final_norm)
```

```python
import math
from contextlib import ExitStack
import numpy as np
import concourse.bass as bass
import concourse.mybir as mybir
from concourse.bass_utils import run_bass_kernel_spmd

F32 = mybir.dt.float32
BF16 = mybir.dt.bfloat16
AF = mybir.ActivationFunctionType
ALU = mybir.AluOpType
AX = mybir.AxisListType

NCORES = 8
T = 1024
D = 2048
KC = 16
DFF = 7168
EPS = 1e-6
LAMBDA_INIT = 0.8 - 0.6 * math.exp(0.0)
MASKVAL = -30000.0
DEBUG = ""


class Sched:
    ENG = ("pe", "act", "dve", "pool", "sp")

    def __init__(self, nc, stack):
        self.nc = nc
        self.stack = stack
        self.streams = {k: [] for k in self.ENG}
        self.sems = {}
        self.cnt = {}
        for k in ("pe", "act", "dve", "pool"):
            self.sems[k] = stack.enter_context(nc.semaphore("prog_" + k))
            self.cnt[k] = 0
        self.last_w = {}
        self.readers = {}
        self.waited = {k: {} for k in self.ENG}
        self.pe_pending = set()

    def _evs(self, reads, writes, acc):
        evs = []
        for k in reads:
            evs += list(self.last_w.get(k, {}).items())
        for k in writes:
            assert k not in self.pe_pending, k
            if not acc:
                evs += list(self.last_w.get(k, {}).items())
                evs += list(self.readers.get(k, {}).items())
        return evs

    def _filter(self, eng, evs):
        out = {}
        for sk, v in evs:
            if eng == "pe" and sk == "pe":
                continue
            if self.waited[eng].get(sk, 0) >= v:
                continue
            if out.get(sk, 0) < v:
                out[sk] = v
        for sk, v in out.items():
            self.waited[eng][sk] = v
        return list(out.items())

    def _record(self, ev, reads, writes, acc):
        for k in reads:
            d = self.readers.setdefault(k, {})
            d[ev[0]] = max(d.get(ev[0], 0), ev[1])
        for k in writes:
            if acc:
                d = self.last_w.setdefault(k, {})
                d[ev[0]] = max(d.get(ev[0], 0), ev[1])
            else:
                self.last_w[k] = {ev[0]: ev[1]}
                self.readers[k] = {}

    def op(self, eng, fn, reads=(), writes=(), acc=False, signal=True):
        waits = self._filter(eng, self._evs(reads, writes, acc))
        if signal:
            self.cnt[eng] += 1
            ev = (eng, self.cnt[eng])
            self.streams[eng].append((fn, waits, ev, 1))
            if eng == "pe" and self.pe_pending:
                self._record(ev, list(self.pe_pending), [], False)
                self.pe_pending = set()
            self._record(ev, reads, writes, acc)
            return ev
        assert eng == "pe"
        self.streams[eng].append((fn, waits, None, 0))
        self.pe_pending.update(reads)
        return None

    def dma(self, eng, out, in_, reads, writes, acc=False, **kw):
        waits = self._filter(eng, self._evs(reads, writes, acc))
        sk = "d_" + writes[0]
        if sk not in self.sems:
            self.sems[sk] = self.stack.enter_context(self.nc.semaphore(sk))
            self.cnt[sk] = 0
        self.cnt[sk] += 16
        ev = (sk, self.cnt[sk])
        self.streams[eng].append((lambda e: e.dma_start(out=out, in_=in_, **kw), waits, ev, 16))
        self._record(ev, reads, writes, acc)
        return ev

    def barrier(self):
        assert not self.pe_pending
        evs = [(k, v) for k, v in self.cnt.items() if v > 0]
        for eng in self.ENG:
            w = self._filter(eng, evs)
            if w:
                self.streams[eng].append((None, w, None, 0))
        self.last_w.clear()
        self.readers.clear()

    def replay(self, eng, e):
        for fn, waits, ev, inc in self.streams[eng]:
            for sk, v in waits:
                e.wait_ge(self.sems[sk], v)
            if fn is None:
                continue
            ins = fn(e)
            if ev is not None:
                ins.then_inc(self.sems[ev[0]], inc)


class Arena:
    def __init__(self, t, nbytes):
        self.t = t
        self.n = nbytes
        self.off = 0

    def seek(self, off):
        self.off = off

    def alloc(self, shape, dt):
        esz = 4 if dt == F32 else 2
        n = int(np.prod(shape)) * esz
        n_al = (n + 63) // 64 * 64
        assert self.off + n_al <= self.n, (self.off, n_al, self.n)
        v = self.t[:, self.off // 2:(self.off + n) // 2]
        self.off += n_al
        if dt == F32:
            v = v.bitcast(F32)
        if len(shape) == 2:
            v = v.rearrange("p (a b) -> p a b", a=shape[0])
        elif len(shape) == 3:
            v = v.rearrange("p (a b c) -> p a b c", a=shape[0], b=shape[1])
        return v


R_X = 0
R_W = 64 * 1024
R_H = 160 * 1024
ARENA_BYTES = 192 * 1024


def build(phases="ABCDE"):
    nc = bass.Bass("TRN2", target_bir_lowering=False)
    first = phases[0]

    def din(name, shape, dt=F32):
        return nc.dram_tensor(name, list(shape), dt, kind="ExternalInput").ap()

    gains_d = din("gains", [128, 5, 16])
    if "A" in phases:
        xh_d = din("xh", [D, 1536])
        xf_d = din("xf", [D, 4096])
        w_in_d = din("w_in", [D, 6144])
        w_sw_d = din("w_sw", [D, 2048])
        w_out0_d = din("w_out0", [D, D])
        ropek_d = din("ropek", [128, 2, 4096])
        ropeq_d = din("ropeq", [128, 2, 1024])
        nab_d = din("nabias", [8, 4, 128, 6, 256])
        lamv_d = din("lamv", [1, 256])
        subln_d = din("subln", [128, 1])
        kscr = nc.dram_tensor("kscr", [8, 128, 4096], BF16).ap()
        vscr = nc.dram_tensor("vscr", [8, 128, 32, 128], BF16).ap()
    if first != "A":
        x0_d = din("x0T", [D, T])
    if "B" in phases:
        ffn_g_d = din("ffn_g", [D, DFF])
        ffn_u_d = din("ffn_u", [D, DFF])
        ffn_d_d = din("ffn_d", [DFF, D])
    if "C" in phases:
        rw_in_d = din("rw_in", [D, 4096])
        rw_out_d = din("rw_out", [D, D])
        lruw_d = din("lruw", [2, 2, 16, 128, 128])
        lrup_d = din("lrup", [128, 5, 2, 16])
        convw_d = din("convw", [128, 16, 5])
        sel_d = din("sel", [128, 4, 8])
        rscr = nc.dram_tensor("rscr", [16, 128, 1024], F32).ap()
        yscr = nc.dram_tensor("yscr", [16, 128, 1024], BF16).ap()
        ex1_in = nc.dram_tensor("ex1_in", [128, 48], F32)
        ex1_out = nc.dram_tensor("ex1_out", [NCORES * 128, 48], F32)
        ex2_in = nc.dram_tensor("ex2_in", [128, 64], F32)
        ex2_out = nc.dram_tensor("ex2_out", [NCORES * 128, 64], F32)
    if "D" in phases:
        router_d = din("router", [128, 16, 8])
        ident_d = din("ident", [128, 128])
        moe_g_d = din("moe_g", [8, D, DFF])
        moe_u_d = din("moe_u", [8, D, DFF])
        moe_d_d = din("moe_d", [8, DFF, D])
    y_d = nc.dram_tensor("y", [D, T], F32, kind="ExternalOutput").ap()

    with ExitStack() as stack:
        S = Sched(nc, stack)
        arena_t = stack.enter_context(nc.sbuf_tensor("arena", [128, ARENA_BYTES // 2], BF16))
        AR = Arena(arena_t, ARENA_BYTES)
        ps = stack.enter_context(nc.psum_tensor("ps", [128, 8, 512], F32))
        ones_bf = stack.enter_context(nc.sbuf_tensor("ones_bf", [128, 128], BF16))
        gains = stack.enter_context(nc.sbuf_tensor("gains_sb", [128, 5, 16], F32))
        epst = stack.enter_context(nc.sbuf_tensor("epst", [128, 1], F32))
        small = stack.enter_context(nc.sbuf_tensor("small", [128, 1024], F32))

        AR.seek(R_X)
        xT = AR.alloc([16, T], F32)
        AR.seek(R_H)
        hbuf = AR.alloc([16, T], BF16)

        S.op("dve", lambda e: e.memset(ones_bf[:], 1.0), writes=["ones"])
        S.op("dve", lambda e: e.memset(epst[:], EPS), writes=["eps"])
        S.dma("sp", gains[:], gains_d, [], ["gains"])

        rot = {"lo": 0, "hi": 0}

        def bank_lo():
            b = rot["lo"] % 4
            rot["lo"] += 1
            return b

        def bank_hi():
            b = 4 + rot["hi"] % 4
            rot["hi"] += 1
            return b

        def mm(bank, n, lhsT, rhs, start, stop, reads, off=0, sig=False):
            S.op("pe", lambda e: e.matmul(ps[:, bank, off:off + n], lhsT, rhs, start=start, stop=stop),
                 reads=reads, writes=["ps%d" % bank], acc=not start, signal=(stop or sig))

        def mm_part(bank, pr, n, lhsT, rhs, start, stop, reads, off=0):
            S.op("pe", lambda e: e.matmul(ps[pr[0]:pr[1], bank, off:off + n], lhsT, rhs, start=start, stop=stop),
                 reads=reads, writes=["ps%d" % bank], acc=not start, signal=stop)

        tmpc = {"i": 0}

        def rmsnorm(src, skey, n, gi, dst, dkey, sq_t, rstd, rkey, dacc=False):
            b = bank_lo()
            sk = (lambda c: skey % c) if "%d" in skey else (lambda c: skey)
            for c in range(KC):
                j = tmpc["i"] % 2
                tmpc["i"] += 1
                S.op("act", lambda e, c=c, j=j: e.activation(out=sq_t[:, j, 0:n], in_=src[:, c, 0:n], func=AF.Square),
                     reads=[sk(c)], writes=["sq%d" % j])
                mm(b, n, ones_bf[:], sq_t[:, j, 0:n], c == 0, c == KC - 1, ["ones", "sq%d" % j], sig=True)
            S.op("act", lambda e: e.activation(out=rstd[:, 0:n], in_=ps[:, b, 0:n], func=AF.Sqrt, bias=epst[:, 0:1], scale=1.0 / D),
                 reads=["ps%d" % b, "eps"], writes=[rkey])
            S.op("dve", lambda e: e.reciprocal(out=rstd[:, 0:n], in_=rstd[:, 0:n]), reads=[rkey], writes=[rkey])
            for c in range(KC):
                S.op("dve", lambda e, c=c: e.scalar_tensor_tensor(out=dst[:, c, 0:n], in0=src[:, c, 0:n], scalar=gains[:, gi, c:c + 1],
                                                                  in1=rstd[:, 0:n], op0=ALU.mult, op1=ALU.mult),
                     reads=[sk(c), rkey, "gains"], writes=[dkey], acc=(dacc or c > 0))

        def wview(w2d, c0, n):
            return w2d.rearrange("(c p) f -> p c f", p=128)[:, :, c0:c0 + n]

        if "A" in phases:
            def phase_A1():
                AR.seek(96 * 1024)
                wk = AR.alloc([16, 1024], BF16)
                wks = AR.alloc([16, 1024], BF16)
                wv = AR.alloc([16, 1024], BF16)
                AR.seek(R_X)
                xs = [AR.alloc([16, 256], F32) for _ in range(2)]
                hTa = [AR.alloc([16, 256], BF16) for _ in range(2)]
                kout = [AR.alloc([8, 256], BF16) for _ in range(2)]
                vout = [AR.alloc([2, 1024], BF16) for _ in range(2)]
                rk = [AR.alloc([2, 256], F32) for _ in range(2)]
                t1 = [AR.alloc([256], F32) for _ in range(2)]
                t2 = [AR.alloc([256], F32) for _ in range(2)]
                sq_t = AR.alloc([2, 512], BF16)
                rstd = [AR.alloc([256], F32) for _ in range(2)]
                for c in range(0, 16, 4):
                    S.dma("pool", wk[:, c:c + 4, :], wview(w_in_d, 4096, 1024)[:, c:c + 4, :], [], ["wk"], acc=c > 0)
                    S.dma("pool", wks[:, c:c + 4, :], wview(w_sw_d, 1024, 1024)[:, c:c + 4, :], [], ["wks"], acc=c > 0)
                    S.dma("pool", wv[:, c:c + 4, :], wview(w_in_d, 5120, 1024)[:, c:c + 4, :], [], ["wv"], acc=c > 0)
                ti = 0
                for tb in range(16):
                    j = tb % 2
                    S.dma("sp", xs[j], xf_d.rearrange("(c p) t -> p c t", p=128)[:, :, tb * 256:(tb + 1) * 256], [], ["xs%d" % j])
                    S.dma("sp", rk[j], ropek_d[:, :, tb * 256:(tb + 1) * 256], [], ["rk%d" % j])
                    rmsnorm(xs[j], "xs%d" % j, 256, 0, hTa[j], "hTa%d" % j, sq_t, rstd[j], "rstd%d" % j)
                    for h in range(8):
                        b1 = bank_lo()
                        for c in range(KC):
                            mm(b1, 256, wk[:, c, h * 128:(h + 1) * 128], hTa[j][:, c, :], c == 0, c == KC - 1, ["wk", "hTa%d" % j])
                        b2 = bank_lo()
                        for c in range(KC):
                            mm(b2, 256, wks[:, c, h * 128:(h + 1) * 128], hTa[j][:, c, :], c == 0, c == KC - 1, ["wks", "hTa%d" % j])
                        q = ti % 2
                        ti += 1
                        S.op("dve", lambda e, b1=b1, q=q, j=j: e.tensor_tensor(out=t1[q], in0=ps[:, b1, 0:256], in1=rk[j][:, 0, :], op=ALU.mult),
                             reads=["ps%d" % b1, "rk%d" % j], writes=["t1_%d" % q])
                        S.op("dve", lambda e, b2=b2, q=q, j=j: e.tensor_tensor(out=t2[q], in0=ps[:, b2, 0:256], in1=rk[j][:, 1, :], op=ALU.mult),
                             reads=["ps%d" % b2, "rk%d" % j], writes=["t2_%d" % q])
                        S.op("dve", lambda e, q=q, j=j, h=h: e.tensor_tensor(out=kout[j][:, h, :], in0=t1[q], in1=t2[q], op=ALU.add),
                             reads=["t1_%d" % q, "t2_%d" % q], writes=["kout%d" % j], acc=h > 0)
                    for tt in range(2):
                        for half in range(2):
                            b = bank_hi()
                            for c in range(KC):
                                mm(b, 512, hTa[j][:, c, tt * 128:(tt + 1) * 128], wv[:, c, half * 512:(half + 1) * 512], c == 0, c == KC - 1,
                                   ["wv", "hTa%d" % j])
                            S.op("act", lambda e, b=b, j=j, tt=tt, half=half: e.activation(out=vout[j][:, tt, half * 512:(half + 1) * 512],
                                                                                             in_=ps[:, b, :], func=AF.Copy),
                                 reads=["ps%d" % b], writes=["vout%d" % j], acc=(tt + half) > 0)
                    S.dma("sp", kscr.rearrange("h p t -> p h t")[:, :, tb * 256:(tb + 1) * 256], kout[j], ["kout%d" % j], ["kscr"], acc=True)
                    for tt in range(2):
                        S.dma("sp", vscr.rearrange("h p k d -> p k h d")[:, tb * 2 + tt, :, :],
                              vout[j][:, tt, :].rearrange("p (h d) -> p h d", h=8), ["vout%d" % j], ["vscr"], acc=True)
                S.barrier()

            phase_A1()
            def phase_A2():
                AR.seek(R_X)
                hTh = AR.alloc([16, 1536], BF16)
                base_a = AR.off
                xs2 = AR.alloc([16, 512], F32)
                sq_t = AR.alloc([2, 512], BF16)
                rstd2 = AR.alloc([512], F32)
                for (o, n) in ((0, 256), (256, 512), (768, 512), (1280, 256)):
                    S.dma("sp", xs2[:, :, 0:n], xh_d.rearrange("(c p) t -> p c t", p=128)[:, :, o:o + n], [], ["xs2"])
                    rmsnorm(xs2, "xs2", n, 0, hTh[:, :, o:o + n], "hTh", sq_t, rstd2, "rstd2", dacc=(o > 0))
                S.barrier()
                return hTh, base_a
            hTh, base_a = phase_A2()
            attnT = hbuf

            def phase_A3(hTh, base_a, attnT):
                AR.seek(base_a)
                wna = [AR.alloc([16, 3, 128], BF16) for _ in range(2)]
                qn = [AR.alloc([1024], BF16) for _ in range(2)]
                kn = [AR.alloc([1536], BF16) for _ in range(2)]
                vn = [AR.alloc([12, 128], BF16) for _ in range(2)]
                nb = [AR.alloc([6, 256], F32) for _ in range(2)]
                sb = [AR.alloc([256], F32) for _ in range(2)]
                pT = [AR.alloc([256], BF16) for _ in range(4)]
                rl = [AR.alloc([256], F32) for _ in range(2)]
                nbi = 0
                pi = 0
                for h in range(8):
                    j = h % 2
                    for w3, c0 in enumerate((h * 128, 1024 + h * 128, 2048 + h * 128)):
                        S.dma("pool", wna[j][:, :, w3, :], wview(w_in_d, c0, 128), [], ["wna%d" % j], acc=w3 > 0)
                    for half in range(2):
                        b = bank_lo()
                        for c in range(KC):
                            mm(b, 512, wna[j][:, c, 0, :], hTh[:, c, 256 + half * 512:256 + (half + 1) * 512], c == 0, c == KC - 1,
                               ["wna%d" % j, "hTh"])
                        S.op("act", lambda e, b=b, j=j, half=half: e.activation(out=qn[j][:, half * 512:(half + 1) * 512], in_=ps[:, b, :],
                                                                                 func=AF.Copy, scale=128.0 ** -0.5),
                             reads=["ps%d" % b], writes=["qn%d" % j], acc=half > 0)
                    for blk in range(3):
                        b = bank_lo()
                        for c in range(KC):
                            mm(b, 512, wna[j][:, c, 1, :], hTh[:, c, blk * 512:(blk + 1) * 512], c == 0, c == KC - 1, ["wna%d" % j, "hTh"])
                        S.op("act", lambda e, b=b, j=j, blk=blk: e.activation(out=kn[j][:, blk * 512:(blk + 1) * 512], in_=ps[:, b, :], func=AF.Copy),
                             reads=["ps%d" % b], writes=["kn%d" % j], acc=blk > 0)
                    for g in range(3):
                        b = bank_lo()
                        for t4 in range(4):
                            tt = g * 4 + t4
                            for c in range(KC):
                                mm(b, 128, hTh[:, c, tt * 128:(tt + 1) * 128], wna[j][:, c, 2, :], c == 0, c == KC - 1, ["wna%d" % j, "hTh"],
                                   off=t4 * 128)
                        S.op("act", lambda e, b=b, j=j, g=g: e.activation(out=vn[j][:, g * 4:(g + 1) * 4, :],
                                                                           in_=ps[:, b, :].rearrange("p (a b) -> p a b", a=4), func=AF.Copy),
                             reads=["ps%d" % b], writes=["vn%d" % j], acc=g > 0)
                    for i in range(4):
                        nj = nbi % 2
                        nbi += 1
                        S.dma("sp", nb[nj], nab_d[h, i], [], ["nb%d" % nj])
                        bo = bank_hi()
                        bl = bank_hi()
                        for s in range(6):
                            kc = 2 * i + s
                            bs = bank_lo()
                            mm(bs, 256, kn[j][:, kc * 128:(kc + 1) * 128], qn[j][:, i * 256:(i + 1) * 256], True, True, ["kn%d" % j, "qn%d" % j])
                            sj = pi % 2
                            pj = pi % 4
                            pi += 1
                            S.op("dve", lambda e, bs=bs, sj=sj, nj=nj, s=s: e.tensor_tensor(out=sb[sj], in0=ps[:, bs, 0:256], in1=nb[nj][:, s, :], op=ALU.add),
                                 reads=["ps%d" % bs, "nb%d" % nj], writes=["sb%d" % sj])
                            S.op("act", lambda e, sj=sj, pj=pj: e.activation(out=pT[pj], in_=sb[sj], func=AF.Exp),
                                 reads=["sb%d" % sj], writes=["pT%d" % pj])
                            mm(bo, 256, vn[j][:, kc, :], pT[pj], s == 0, s == 5, ["vn%d" % j, "pT%d" % pj])
                            mm(bl, 256, ones_bf[:], pT[pj], s == 0, s == 5, ["ones", "pT%d" % pj], sig=True)
                        rj = i % 2
                        S.op("dve", lambda e, bl=bl, rj=rj: e.reciprocal(out=rl[rj], in_=ps[:, bl, 0:256]), reads=["ps%d" % bl], writes=["rl%d" % rj])
                        S.op("dve", lambda e, bo=bo, rj=rj, h=h, i=i: e.tensor_tensor(out=attnT[:, h, i * 256:(i + 1) * 256], in0=ps[:, bo, 0:256],
                                                                                      in1=rl[rj], op=ALU.mult),
                             reads=["ps%d" % bo, "rl%d" % rj], writes=["attnT"], acc=True)
                S.barrier()

            phase_A3(hTh, base_a, attnT)
            def phase_A4(hTh, base_a, attnT):
                AR.seek(base_a)
                wda = [AR.alloc([16, 2, 128], BF16) for _ in range(2)]
                rq = AR.alloc([2, 1024], F32)
                qd = [AR.alloc([1024], BF16) for _ in range(2)]
                kd = [AR.alloc([4096], BF16) for _ in range(2)]
                vd = [AR.alloc([32, 128], BF16) for _ in range(2)]
                pT = [AR.alloc([512], BF16) for _ in range(4)]
                t1 = [AR.alloc([512], F32) for _ in range(2)]
                t2 = [AR.alloc([512], F32) for _ in range(2)]
                r0 = AR.alloc([512], F32)
                r1 = AR.alloc([512], F32)
                o0 = AR.alloc([512], F32)
                o1 = AR.alloc([512], F32)
                oo = AR.alloc([512], F32)
                sqd = AR.alloc([512], BF16)
                rsd = AR.alloc([512], F32)
                lam_t = AR.alloc([256], F32)
                lam_s = AR.alloc([8], F32)
                subl = AR.alloc([2], F32)
                S.dma("sp", rq, ropeq_d, [], ["rq"])
                S.dma("sp", lam_t, lamv_d.partition_broadcast(128).rearrange("p a b -> p (a b)"), [], ["lam_t"])
                S.dma("sp", subl[:, 0:1], subln_d, [], ["subl"])
                S.op("dve", lambda e: e.tensor_tensor(out=lam_t[:, 0:64], in0=lam_t[:, 0:64], in1=lam_t[:, 64:128], op=ALU.mult), reads=["lam_t"], writes=["lam_t"])
                S.op("dve", lambda e: e.tensor_tensor(out=lam_t[:, 128:192], in0=lam_t[:, 128:192], in1=lam_t[:, 192:256], op=ALU.mult), reads=["lam_t"], writes=["lam_t"])
                S.op("dve", lambda e: e.reduce_sum(out=lam_s[:, 0:1], in_=lam_t[:, 0:64], axis=AX.X), reads=["lam_t"], writes=["lam_s"])
                S.op("dve", lambda e: e.reduce_sum(out=lam_s[:, 1:2], in_=lam_t[:, 128:192], axis=AX.X), reads=["lam_t"], writes=["lam_s"])
                S.op("act", lambda e: e.activation(out=lam_s[:, 2:4], in_=lam_s[:, 0:2], func=AF.Exp), reads=["lam_s"], writes=["lam_s"])
                S.op("dve", lambda e: e.tensor_tensor(out=lam_s[:, 4:5], in0=lam_s[:, 3:4], in1=lam_s[:, 2:3], op=ALU.subtract), reads=["lam_s"], writes=["lam_s"])
                S.op("dve", lambda e: e.tensor_scalar(out=lam_s[:, 5:6], in0=lam_s[:, 4:5], scalar1=-LAMBDA_INIT, scalar2=None, op0=ALU.add), reads=["lam_s"], writes=["lam_s"])
                S.op("dve", lambda e: e.tensor_scalar(out=subl[:, 1:2], in0=subl[:, 0:1], scalar1=1.0 - LAMBDA_INIT, scalar2=None, op0=ALU.mult), reads=["subl"], writes=["subl"])
                pi = 0
                ti = 0
                for h in range(8):
                    j = h % 2
                    S.dma("pool", wda[j][:, :, 0, :], wview(w_in_d, 3072 + h * 128, 128), [], ["wda%d" % j])
                    S.dma("pool", wda[j][:, :, 1, :], wview(w_sw_d, h * 128, 128), [], ["wda%d" % j], acc=True)
                    S.dma("sp", kd[j], kscr[h], [], ["kd%d" % j])
                    S.dma("sp", vd[j], vscr[h], [], ["vd%d" % j])
                    for half in range(2):
                        b1 = bank_lo()
                        for c in range(KC):
                            mm(b1, 512, wda[j][:, c, 0, :], hTh[:, c, 256 + half * 512:256 + (half + 1) * 512], c == 0, c == KC - 1, ["wda%d" % j, "hTh"])
                        b2 = bank_lo()
                        for c in range(KC):
                            mm(b2, 512, wda[j][:, c, 1, :], hTh[:, c, 256 + half * 512:256 + (half + 1) * 512], c == 0, c == KC - 1, ["wda%d" % j, "hTh"])
                        q = ti % 2
                        ti += 1
                        S.op("dve", lambda e, b1=b1, q=q, half=half: e.scalar_tensor_tensor(out=t1[q], in0=ps[:, b1, :], scalar=0.125,
                                                                                            in1=rq[:, 0, half * 512:(half + 1) * 512], op0=ALU.mult, op1=ALU.mult),
                             reads=["ps%d" % b1, "rq"], writes=["t1_%d" % q])
                        S.op("dve", lambda e, b2=b2, q=q, half=half: e.scalar_tensor_tensor(out=t2[q], in0=ps[:, b2, :], scalar=0.125,
                                                                                            in1=rq[:, 1, half * 512:(half + 1) * 512], op0=ALU.mult, op1=ALU.mult),
                             reads=["ps%d" % b2, "rq"], writes=["t2_%d" % q])
                        S.op("dve", lambda e, q=q, j=j, half=half: e.tensor_tensor(out=qd[j][:, half * 512:(half + 1) * 512], in0=t1[q], in1=t2[q], op=ALU.add),
                             reads=["t1_%d" % q, "t2_%d" % q], writes=["qd%d" % j], acc=half > 0)
                    for qh in range(2):
                        steps = [(kc, c) for kc in range(32) for c in range(2)]
                        sbank = {}

                        def emit_s(idx):
                            kc, c = steps[idx]
                            bs = bank_lo()
                            sbank[idx] = bs
                            mm(bs, 512, kd[j][64 * c:64 * c + 64, kc * 128:(kc + 1) * 128], qd[j][64 * c:64 * c + 64, qh * 512:(qh + 1) * 512], True, True,
                               ["kd%d" % j, "qd%d" % j])

                        emit_s(0)
                        emit_s(1)
                        for idx in range(64):
                            kc, c = steps[idx]
                            bs = sbank[idx]
                            pj = pi % 4
                            pi += 1
                            S.op("act", lambda e, bs=bs, pj=pj: e.activation(out=pT[pj], in_=ps[:, bs, :], func=AF.Exp), reads=["ps%d" % bs], writes=["pT%d" % pj])
                            if idx + 2 < 64:
                                emit_s(idx + 2)
                            mm(4 + c, 512, vd[j][:, kc, :], pT[pj], kc == 0, kc == 31, ["vd%d" % j, "pT%d" % pj])
                            mm(6 + c, 512, ones_bf[:], pT[pj], kc == 0, kc == 31, ["ones", "pT%d" % pj], sig=True)
                        S.op("dve", lambda e: e.reciprocal(out=r0, in_=ps[:, 6, :]), reads=["ps6"], writes=["r0"])
                        S.op("dve", lambda e: e.reciprocal(out=r1, in_=ps[:, 7, :]), reads=["ps7"], writes=["r1"])
                        S.op("dve", lambda e: e.tensor_tensor(out=o0, in0=ps[:, 4, :], in1=r0, op=ALU.mult), reads=["ps4", "r0"], writes=["o0"])
                        S.op("dve", lambda e: e.tensor_tensor(out=o1, in0=ps[:, 5, :], in1=r1, op=ALU.mult), reads=["ps5", "r1"], writes=["o1"])
                        S.op("dve", lambda e: e.scalar_tensor_tensor(out=oo, in0=o1, scalar=lam_s[:, 5:6], in1=o0, op0=ALU.mult, op1=ALU.add),
                             reads=["o0", "o1", "lam_s"], writes=["oo"])
                        S.op("act", lambda e: e.activation(out=sqd, in_=oo, func=AF.Square), reads=["oo"], writes=["sqd"])
                        bn = bank_lo()
                        mm(bn, 512, ones_bf[:], sqd, True, True, ["ones", "sqd"])
                        S.op("act", lambda e, bn=bn: e.activation(out=rsd, in_=ps[:, bn, :], func=AF.Sqrt, bias=epst[:, 0:1], scale=1.0 / 128),
                             reads=["ps%d" % bn, "eps"], writes=["rsd"])
                        S.op("dve", lambda e: e.reciprocal(out=rsd, in_=rsd), reads=["rsd"], writes=["rsd"])
                        S.op("dve", lambda e, h=h, qh=qh: e.scalar_tensor_tensor(out=attnT[:, 8 + h, qh * 512:(qh + 1) * 512], in0=oo, scalar=subl[:, 1:2],
                                                                                 in1=rsd, op0=ALU.mult, op1=ALU.mult),
                             reads=["oo", "rsd", "subl"], writes=["attnT"], acc=True)
                S.barrier()

            phase_A4(hTh, base_a, attnT)
        if first == "A":
            S.dma("sp", xT, xh_d.rearrange("(c p) t -> p c t", p=128)[:, :, 256:1280], [], ["xT%d" % m_ for m_ in range(16)])
        else:
            S.dma("sp", xT, x0_d.rearrange("(c p) t -> p c t", p=128), [], ["xT%d" % m_ for m_ in range(16)])

        def out_proj(w_d, src, skey):
            AR.seek(R_W)
            wo = [AR.alloc([16, 512], BF16) for _ in range(2)]
            for mg in range(4):
                j = mg % 2
                S.dma("pool", wo[j], wview(w_d, mg * 512, 512), [], ["wo%d" % j])
                for m in range(4):
                    for half in range(2):
                        b = bank_hi()
                        for c in range(KC):
                            mm(b, 512, wo[j][:, c, m * 128:(m + 1) * 128], src[:, c, half * 512:(half + 1) * 512], c == 0, c == KC - 1, ["wo%d" % j, skey])
                        mt = mg * 4 + m
                        S.op("dve", lambda e, b=b, mt=mt, half=half: e.tensor_tensor(out=xT[:, mt, half * 512:(half + 1) * 512], in0=ps[:, b, :],
                                                                                       in1=xT[:, mt, half * 512:(half + 1) * 512], op=ALU.add),
                             reads=["ps%d" % b, "xT%d" % mt], writes=["xT%d" % mt])
            S.barrier()

        if "A" in phases and DEBUG != "attn":
            out_proj(w_out0_d, hbuf, "attnT")

        def ffn_pass(wg_d, wu_d, wd_d, hT, wgu, wds, actT, sg, tmpu, cb):
            st = {"n": 0}

            def load(fg):
                j = fg % 2
                S.dma("pool", wgu[j][:, :, 0, :], wview(wg_d, fg * 256, 256), [], ["wgu%d" % j])
                S.dma("pool", wgu[j][:, :, 1, :], wview(wu_d, fg * 256, 256), [], ["wgu%d" % j], acc=True)
                S.dma("pool", wds[j], wd_d[fg * 256:(fg + 1) * 256, :].rearrange("(j p) n -> p j n", p=128), [], ["wds%d" % j])

            def gu(fg):
                j = fg % 2
                for ft in range(2):
                    for th in range(2):
                        bg = bank_lo()
                        for c in range(KC):
                            mm(bg, 512, wgu[j][:, c, 0, ft * 128:(ft + 1) * 128], hT[:, c, th * 512:(th + 1) * 512], c == 0, c == KC - 1, ["wgu%d" % j, "hT"])
                        bu = bank_lo()
                        for c in range(KC):
                            mm(bu, 512, wgu[j][:, c, 1, ft * 128:(ft + 1) * 128], hT[:, c, th * 512:(th + 1) * 512], c == 0, c == KC - 1, ["wgu%d" % j, "hT"])
                        q = st["n"] % 2
                        st["n"] += 1
                        S.op("act", lambda e, bg=bg, q=q: e.activation(out=sg[q], in_=ps[:, bg, :], func=AF.Silu), reads=["ps%d" % bg], writes=["sg%d" % q])
                        akey = "act%d_%d" % (j, ft * 2 + th)
                        if cb is None:
                            S.op("dve", lambda e, bu=bu, q=q, j=j, ft=ft, th=th: e.tensor_tensor(out=actT[j][:, ft, th * 512:(th + 1) * 512], in0=ps[:, bu, :],
                                                                                                 in1=sg[q], op=ALU.mult),
                                 reads=["ps%d" % bu, "sg%d" % q], writes=[akey])
                        else:
                            S.op("dve", lambda e, bu=bu, q=q, th=th: e.tensor_tensor(out=tmpu[q], in0=ps[:, bu, :], in1=cb[:, th * 512:(th + 1) * 512], op=ALU.mult),
                                 reads=["ps%d" % bu, "cb"], writes=["tmpu%d" % q])
                            S.op("dve", lambda e, q=q, j=j, ft=ft, th=th: e.tensor_tensor(out=actT[j][:, ft, th * 512:(th + 1) * 512], in0=tmpu[q], in1=sg[q], op=ALU.mult),
                                 reads=["tmpu%d" % q, "sg%d" % q], writes=[akey])

            def down(fg):
                j = fg % 2
                for m in range(16):
                    for th in range(2):
                        b = bank_hi()
                        for ft in range(2):
                            mm(b, 512, wds[j][:, ft, m * 128:(m + 1) * 128], actT[j][:, ft, th * 512:(th + 1) * 512], ft == 0, ft == 1,
                               ["wds%d" % j, "act%d_%d" % (j, ft * 2 + th)])
                        S.op("dve", lambda e, b=b, m=m, th=th: e.tensor_tensor(out=xT[:, m, th * 512:(th + 1) * 512], in0=ps[:, b, :],
                                                                                 in1=xT[:, m, th * 512:(th + 1) * 512], op=ALU.add),
                             reads=["ps%d" % b, "xT%d" % m], writes=["xT%d" % m])

            NFG = DFF // 256
            load(0)
            load(1)
            gu(0)
            for fg in range(NFG):
                if fg + 1 < NFG:
                    gu(fg + 1)
                down(fg)
                if fg + 2 < NFG:
                    load(fg + 2)

        def ffn_bufs():
            AR.seek(R_W)
            wgu = [AR.alloc([16, 2, 256], BF16) for _ in range(2)]
            wds = [AR.alloc([2, 2048], BF16) for _ in range(2)]
            actT = [AR.alloc([2, 1024], BF16) for _ in range(2)]
            sg = [AR.alloc([512], F32) for _ in range(2)]
            tmpu = [AR.alloc([512], F32) for _ in range(2)]
            sq_t = AR.alloc([2, 512], BF16)
            rstd = AR.alloc([1024], F32)
            return wgu, wds, actT, sg, tmpu, sq_t, rstd

        def norm_x(gi, dst, dkey, sq_t, rstd):
            for half in range(2):
                rmsnorm(xT[:, :, half * 512:(half + 1) * 512], "xT%d", 512, gi, dst[:, :, half * 512:(half + 1) * 512], dkey, sq_t,
                        rstd[:, half * 512:(half + 1) * 512], "rstd", dacc=half > 0)

        if "B" in phases:
            wgu, wds, actT, sg, tmpu, sq_t, rstd = ffn_bufs()
            norm_x(1, hbuf, "hT", sq_t, rstd)
            ffn_pass(ffn_g_d, ffn_u_d, ffn_d_d, hbuf, wgu, wds, actT, sg, tmpu, None)
            S.barrier()

        if "C" in phases:
            AR.seek(R_W)
            edge = AR.alloc([16, 3], F32)
            g1 = AR.alloc([8, 48], F32)
            accL = AR.alloc([48], F32)
            accR = AR.alloc([48], F32)
            lrup = AR.alloc([5, 2, 16], F32)
            convw = AR.alloc([16, 5], F32)
            sel = AR.alloc([4, 8], F32)
            spv = AR.alloc([4, 32], F32)
            ex2 = AR.alloc([4, 16], F32)
            g2 = AR.alloc([8, 64], F32)
            car = AR.alloc([2, 16], F32)
            ctmp = AR.alloc([16], F32)
            sumga = AR.alloc([2, 16, 2], F32)
            base_c = AR.off
            wi = [AR.alloc([16, 512], BF16) for _ in range(2)]
            sq_t = AR.alloc([2, 512], BF16)
            rstd = AR.alloc([1024], F32)
            g1t = [AR.alloc([512], F32) for _ in range(2)]
            g2t = [AR.alloc([512], F32) for _ in range(2)]
            yt = [AR.alloc([512], BF16) for _ in range(2)]
            rt = [AR.alloc([512], F32) for _ in range(2)]
            S.dma("sp", lrup, lrup_d, [], ["lrup"])
            S.dma("sp", convw, convw_d, [], ["convw"])
            S.dma("sp", sel, sel_d, [], ["sel"])
            norm_x(2, hbuf, "hT", sq_t, rstd)
            lv = lrup[:, 2, :, :].rearrange("p a b -> p (a b)")
            S.op("dve", lambda e: e.tensor_scalar(out=spv[:, 0, :], in0=lv, scalar1=-1.0, scalar2=None, op0=ALU.mult), reads=["lrup"], writes=["spv"])
            S.op("dve", lambda e: e.tensor_tensor(out=spv[:, 1, :], in0=spv[:, 0, :], in1=lv, op=ALU.max), reads=["spv", "lrup"], writes=["spv"])
            S.op("act", lambda e: e.activation(out=spv[:, 2, :], in_=spv[:, 1, :], func=AF.Exp, scale=-1.0), reads=["spv"], writes=["spv"])
            S.op("act", lambda e: e.activation(out=spv[:, 2, :], in_=spv[:, 2, :], func=AF.Ln, bias=1.0), reads=["spv"], writes=["spv"])
            S.op("dve", lambda e: e.tensor_scalar(out=spv[:, 1, :], in0=spv[:, 0, :], scalar1=0.0, scalar2=None, op0=ALU.max), reads=["spv"], writes=["spv"])
            S.op("dve", lambda e: e.tensor_tensor(out=spv[:, 3, :], in0=spv[:, 1, :], in1=spv[:, 2, :], op=ALU.add), reads=["spv"], writes=["spv"])
            S.op("dve", lambda e: e.tensor_scalar(out=spv[:, 0, :], in0=spv[:, 3, :], scalar1=-8.0, scalar2=None, op0=ALU.mult), reads=["spv"], writes=["spv"])
            gi2 = 0
            for cg in range(8):
                j = cg % 2
                S.dma("pool", wi[j], wview(rw_in_d, cg * 512, 512), [], ["wi%d" % j])
                for m in range(4):
                    for half in range(2):
                        b = bank_lo()
                        for c in range(KC):
                            mm(b, 512, wi[j][:, c, m * 128:(m + 1) * 128], hbuf[:, c, half * 512:(half + 1) * 512], c == 0, c == KC - 1, ["wi%d" % j, "hT"])
                        q = gi2 % 2
                        gi2 += 1
                        if cg < 4:
                            n = cg * 4 + m
                            S.op("act", lambda e, b=b, q=q: e.activation(out=g1t[q], in_=ps[:, b, :], func=AF.Square), reads=["ps%d" % b], writes=["g1t%d" % q])
                            S.op("dve", lambda e, q=q: e.tensor_scalar(out=g1t[q], in0=g1t[q], scalar1=0.044715, scalar2=1.0, op0=ALU.mult, op1=ALU.add),
                                 reads=["g1t%d" % q], writes=["g1t%d" % q])
                            S.op("dve", lambda e, b=b, q=q: e.tensor_tensor(out=g1t[q], in0=ps[:, b, :], in1=g1t[q], op=ALU.mult),
                                 reads=["ps%d" % b, "g1t%d" % q], writes=["g1t%d" % q])
                            S.op("act", lambda e, q=q: e.activation(out=g2t[q], in_=g1t[q], func=AF.Sigmoid, scale=2.0 * math.sqrt(2.0 / math.pi)),
                                 reads=["g1t%d" % q], writes=["g2t%d" % q])
                            S.op("dve", lambda e, b=b, q=q: e.tensor_tensor(out=yt[q], in0=ps[:, b, :], in1=g2t[q], op=ALU.mult),
                                 reads=["ps%d" % b, "g2t%d" % q], writes=["yt%d" % q])
                            S.dma("sp", yscr[n][:, half * 512:(half + 1) * 512], yt[q], ["yt%d" % q], ["yscr"], acc=True)
                        else:
                            n = (cg - 4) * 4 + m
                            S.op("act", lambda e, b=b, q=q: e.activation(out=rt[q], in_=ps[:, b, :], func=AF.Copy), reads=["ps%d" % b], writes=["rt%d" % q])
                            if half == 0:
                                S.op("dve", lambda e, q=q, n=n: e.tensor_copy(out=edge[:, n, 0:2], in_=rt[q][:, 0:2]), reads=["rt%d" % q], writes=["edge"], acc=True)
                            else:
                                S.op("dve", lambda e, q=q, n=n: e.tensor_copy(out=edge[:, n, 2:3], in_=rt[q][:, 511:512]), reads=["rt%d" % q], writes=["edge"], acc=True)
                            S.dma("sp", rscr[n][:, half * 512:(half + 1) * 512], rt[q], ["rt%d" % q], ["rscr"], acc=True)
            S.dma("pool", ex1_in.ap(), edge.rearrange("p a b -> p (a b)"), ["edge"], ["ex1_in"])
            waits = S._filter("pool", S._evs(["ex1_in"], ["ex1_out"], False))
            S.sems["cc"] = stack.enter_context(nc.semaphore("cc"))
            S.cnt["cc"] = 1
            S.streams["pool"].append((lambda e: e.collective_compute("AllGather", ALU.bypass, replica_groups=[list(range(NCORES))],
                                                                     ins=[ex1_in.ap().opt()], outs=[ex1_out.ap().opt()]), waits, ("cc", 1), 1))
            S._record(("cc", 1), ["ex1_in"], ["ex1_out"], False)
            S.dma("pool", g1, ex1_out.ap().rearrange("(r p) f -> p r f", p=128), ["ex1_out"], ["g1"])
            for r in range(8):
                for acc_t, si, key in ((accL, 0, "accL"), (accR, 1, "accR")):
                    if r == 0:
                        S.op("dve", lambda e, acc_t=acc_t, si=si, r=r: e.tensor_scalar(out=acc_t, in0=g1[:, r, :], scalar1=sel[:, si, r:r + 1], scalar2=None, op0=ALU.mult),
                             reads=["g1", "sel"], writes=[key])
                    else:
                        S.op("dve", lambda e, acc_t=acc_t, si=si, r=r: e.scalar_tensor_tensor(out=acc_t, in0=g1[:, r, :], scalar=sel[:, si, r:r + 1], in1=acc_t,
                                                                                               op0=ALU.mult, op1=ALU.add),
                             reads=["g1", "sel", key], writes=[key])
            S.barrier()

            AR.seek(base_c)
            lw4 = AR.alloc([4, 16, 128], BF16)
            recx = AR.alloc([1032], F32)
            u = AR.alloc([1024], F32)
            ub = AR.alloc([1024], BF16)
            ga = AR.alloc([512], F32)
            gx = AR.alloc([512], F32)
            a_t = [AR.alloc([1024], F32) for _ in range(2)]
            b_t = [AR.alloc([1024], F32) for _ in range(2)]
            hs = [AR.alloc([1024], F32) for _ in range(2)]
            ybf = AR.alloc([1024], BF16)
            for ax in range(2):
                for dr in range(2):
                    S.dma("pool", lw4[:, ax * 2 + dr, :, :], lruw_d[ax, dr].rearrange("n i j -> i n j"), [], ["lw"], acc=(ax + dr) > 0)

            def lru_chunk(n, final):
                S.dma("sp", recx[:, 1:1025], rscr[n], [], ["recx"])
                S.op("dve", lambda e: e.tensor_copy(out=recx[:, 0:1], in_=accL[:, n * 3 + 2:n * 3 + 3]), reads=["accL"], writes=["recx"], acc=True)
                S.op("dve", lambda e: e.tensor_copy(out=recx[:, 1025:1027], in_=accR[:, n * 3:n * 3 + 2]), reads=["accR"], writes=["recx"], acc=True)
                S.op("dve", lambda e: e.tensor_scalar(out=u, in0=recx[:, 0:1024], scalar1=convw[:, n, 0:1], scalar2=convw[:, n, 4:5], op0=ALU.mult, op1=ALU.add),
                     reads=["recx", "convw"], writes=["u"])
                for k in range(1, 4):
                    S.op("dve", lambda e, k=k: e.scalar_tensor_tensor(out=u, in0=recx[:, k:k + 1024], scalar=convw[:, n, k:k + 1], in1=u, op0=ALU.mult, op1=ALU.add),
                         reads=["recx", "convw", "u"], writes=["u"])
                S.op("act", lambda e: e.activation(out=ub, in_=u, func=AF.Copy), reads=["u"], writes=["ub"])
                for dr in range(2):
                    for half in range(2):
                        sl = slice(half * 512, (half + 1) * 512)
                        ba = bank_lo()
                        mm(ba, 512, lw4[:, 0 * 2 + dr, n, :], ub[:, sl], True, True, ["lw", "ub"])
                        bx = bank_lo()
                        mm(bx, 512, lw4[:, 1 * 2 + dr, n, :], ub[:, sl], True, True, ["lw", "ub"])
                        S.op("act", lambda e, ba=ba, dr=dr, half=half: e.activation(out=ga, in_=ps[:, ba, :], func=AF.Sigmoid, bias=lrup[:, 0, dr, n:n + 1],
                                                                                    accum_out=sumga[:, dr, n, half:half + 1]),
                             reads=["ps%d" % ba, "lrup"], writes=["ga", "sumga"])
                        S.op("act", lambda e, bx=bx, dr=dr: e.activation(out=gx, in_=ps[:, bx, :], func=AF.Sigmoid, bias=lrup[:, 1, dr, n:n + 1]),
                             reads=["ps%d" % bx, "lrup"], writes=["gx"])
                        S.op("act", lambda e, dr=dr, sl=sl: e.activation(out=a_t[dr][:, sl], in_=ga, func=AF.Exp, scale=spv[:, 0, dr * 16 + n:dr * 16 + n + 1]),
                             reads=["ga", "spv"], writes=["a%d" % dr], acc=half > 0)
                        S.op("dve", lambda e, dr=dr, sl=sl: e.tensor_tensor(out=ga, in0=a_t[dr][:, sl], in1=a_t[dr][:, sl], op=ALU.mult),
                             reads=["a%d" % dr], writes=["ga"])
                        S.op("act", lambda e: e.activation(out=ga, in_=ga, func=AF.Sqrt, bias=1.0, scale=-1.0), reads=["ga"], writes=["ga"])
                        S.op("dve", lambda e, sl=sl: e.tensor_tensor(out=gx, in0=gx, in1=u[:, sl], op=ALU.mult), reads=["gx", "u"], writes=["gx"])
                        S.op("dve", lambda e, dr=dr, sl=sl: e.tensor_tensor(out=b_t[dr][:, sl], in0=gx, in1=ga, op=ALU.mult),
                             reads=["gx", "ga"], writes=["b%d" % dr], acc=half > 0)
                init_f = car[:, 0, n:n + 1] if final else 0.0
                init_b = car[:, 1, n:n + 1] if final else 0.0
                S.op("dve", lambda e: e.tensor_tensor_scan(out=hs[0], data0=a_t[0], data1=b_t[0], initial=init_f, op0=ALU.mult, op1=ALU.add),
                     reads=["a0", "b0", "car"], writes=["hs0"])
                S.op("dve", lambda e: e.tensor_tensor_scan(out=hs[1][:, ::-1], data0=a_t[1][:, ::-1], data1=b_t[1][:, ::-1], initial=init_b, op0=ALU.mult, op1=ALU.add),
                     reads=["a1", "b1", "car"], writes=["hs1"])
                if not final:
                    S.op("dve", lambda e: e.tensor_copy(out=ex2[:, 0, n:n + 1], in_=hs[0][:, 1023:1024]), reads=["hs0"], writes=["ex2"], acc=True)
                    S.op("dve", lambda e: e.tensor_copy(out=ex2[:, 2, n:n + 1], in_=hs[1][:, 0:1]), reads=["hs1"], writes=["ex2"], acc=True)
                else:
                    S.dma("sp", ybf, yscr[n], [], ["ybf"])
                    S.op("dve", lambda e: e.tensor_tensor(out=hs[0], in0=hs[0], in1=hs[1], op=ALU.add), reads=["hs0", "hs1"], writes=["hs0"])
                    S.op("dve", lambda e: e.tensor_tensor(out=hbuf[:, n, :], in0=hs[0], in1=ybf, op=ALU.mult), reads=["hs0", "ybf"], writes=["zT"], acc=True)

            for n in range(16):
                lru_chunk(n, False)
            for dr in range(2):
                S.op("dve", lambda e, dr=dr: e.tensor_tensor(out=ctmp, in0=sumga[:, dr, :, 0], in1=sumga[:, dr, :, 1], op=ALU.add), reads=["sumga"], writes=["ctmp"])
                S.op("dve", lambda e, dr=dr: e.tensor_tensor(out=ctmp, in0=ctmp, in1=spv[:, 0, dr * 16:(dr + 1) * 16], op=ALU.mult), reads=["ctmp", "spv"], writes=["ctmp"])
                S.op("act", lambda e, dr=dr: e.activation(out=ex2[:, 1 + 2 * dr, :], in_=ctmp, func=AF.Exp), reads=["ctmp"], writes=["ex2"], acc=True)
            S.dma("pool", ex2_in.ap(), ex2.rearrange("p a b -> p (a b)"), ["ex2"], ["ex2_in"])
            waits = S._filter("pool", S._evs(["ex2_in"], ["ex2_out"], False))
            S.cnt["cc"] = 2
            S.streams["pool"].append((lambda e: e.collective_compute("AllGather", ALU.bypass, replica_groups=[list(range(NCORES))],
                                                                     ins=[ex2_in.ap().opt()], outs=[ex2_out.ap().opt()]), waits, ("cc", 2), 1))
            S._record(("cc", 2), ["ex2_in"], ["ex2_out"], False)
            S.dma("pool", g2, ex2_out.ap().rearrange("(r p) f -> p r f", p=128), ["ex2_out"], ["g2"])
            S.op("dve", lambda e: e.memset(car.rearrange("p a b -> p (a b)"), 0.0), writes=["car"])
            for dr, order, mi in ((0, range(8), 2), (1, range(7, -1, -1), 3)):
                for r in order:
                    Hr = g2[:, r, dr * 32:dr * 32 + 16]
                    Ar = g2[:, r, dr * 32 + 16:dr * 32 + 32]
                    S.op("dve", lambda e, Ar=Ar, dr=dr: e.tensor_tensor(out=ctmp, in0=Ar, in1=car[:, dr, :], op=ALU.mult), reads=["g2", "car"], writes=["ctmp"])
                    S.op("dve", lambda e, Hr=Hr: e.tensor_tensor(out=ctmp, in0=ctmp, in1=Hr, op=ALU.add), reads=["g2", "ctmp"], writes=["ctmp"])
                    S.op("dve", lambda e, dr=dr: e.tensor_tensor(out=ctmp, in0=ctmp, in1=car[:, dr, :], op=ALU.subtract), reads=["car", "ctmp"], writes=["ctmp"])
                    S.op("dve", lambda e, dr=dr, r=r, mi=mi: e.scalar_tensor_tensor(out=car[:, dr, :], in0=ctmp, scalar=sel[:, mi, r:r + 1], in1=car[:, dr, :],
                                                                                    op0=ALU.mult, op1=ALU.add),
                         reads=["ctmp", "sel", "car"], writes=["car"])
            for n in range(16):
                lru_chunk(n, True)
            S.barrier()
            out_proj(rw_out_d, hbuf, "zT")

        if "D" in phases:
            wgu, wds, actT, sg, tmpu, sq_t, rstd = ffn_bufs()
            cb = AR.alloc([1024], F32)
            gr = AR.alloc([16, 8], F32)
            rt_sb = AR.alloc([16, 8], F32)
            ident = AR.alloc([128], F32)
            lg = AR.alloc([8, 8], F32)
            comb = AR.alloc([8, 8], F32)
            tk = AR.alloc([8, 8], F32)
            m12 = AR.alloc([16], F32)
            dg = [AR.alloc([128], F32) for _ in range(2)]
            rsT = AR.alloc([8, 8], F32)
            onesf = AR.alloc([128], F32)
            S.dma("sp", rt_sb, router_d, [], ["rt_sb"])
            S.dma("sp", ident, ident_d, [], ["ident"])
            S.op("dve", lambda e: e.memset(onesf, 1.0), writes=["onesf"])
            norm_x(4, hbuf, "hT", sq_t, rstd)
            for ex_ in range(8):
                S.op("dve", lambda e, ex_=ex_: e.tensor_tensor(out=gr[:, :, ex_], in0=rt_sb[:, :, ex_], in1=gains[:, 4, :], op=ALU.mult),
                     reads=["rt_sb", "gains"], writes=["gr"], acc=ex_ > 0)
            for tt in range(8):
                b = bank_lo()
                for c in range(KC):
                    mm(b, 8, xT[:, c, tt * 128:(tt + 1) * 128], gr[:, c, :], c == 0, c == KC - 1, ["xT%d" % c, "gr"])
                b2 = bank_lo()
                mm(b2, 8, rstd[0:1, tt * 128:(tt + 1) * 128], onesf[0:1, 0:8], True, True, ["rstd", "onesf"])
                S.op("act", lambda e, b2=b2, tt=tt: e.activation(out=rsT[:, tt, :], in_=ps[:, b2, 0:8], func=AF.Copy), reads=["ps%d" % b2], writes=["rsT"], acc=tt > 0)
                S.op("dve", lambda e, b=b, tt=tt: e.tensor_tensor(out=lg[:, tt, :], in0=ps[:, b, 0:8], in1=rsT[:, tt, :], op=ALU.mult),
                     reads=["ps%d" % b, "rsT"], writes=["lg"], acc=tt > 0)
            for tt in range(8):
                L = lg[:, tt, :]
                M1 = tk[:, tt, :]
                S.op("dve", lambda e, L=L: e.reduce_max(out=m12[:, 0:1], in_=L, axis=AX.X), reads=["lg"], writes=["m12"])
                S.op("dve", lambda e, L=L, M1=M1: e.tensor_scalar(out=M1, in0=L, scalar1=m12[:, 0:1], scalar2=None, op0=ALU.is_equal), reads=["lg", "m12"], writes=["tk"])
                S.op("dve", lambda e, L=L, M1=M1, tt=tt: e.scalar_tensor_tensor(out=comb[:, tt, :], in0=M1, scalar=-1e30, in1=L, op0=ALU.mult, op1=ALU.add),
                     reads=["tk", "lg"], writes=["comb"])
                S.op("dve", lambda e, tt=tt: e.reduce_max(out=m12[:, 1:2], in_=comb[:, tt, :], axis=AX.X), reads=["comb"], writes=["m12"])
                S.op("dve", lambda e, tt=tt: e.tensor_scalar(out=comb[:, tt, :], in0=comb[:, tt, :], scalar1=m12[:, 1:2], scalar2=None, op0=ALU.is_equal),
                     reads=["comb", "m12"], writes=["comb"])
                S.op("dve", lambda e: e.tensor_tensor(out=m12[:, 2:3], in0=m12[:, 1:2], in1=m12[:, 0:1], op=ALU.subtract), reads=["m12"], writes=["m12"])
                S.op("act", lambda e: e.activation(out=m12[:, 3:4], in_=m12[:, 2:3], func=AF.Exp), reads=["m12"], writes=["m12"])
                S.op("dve", lambda e: e.tensor_scalar(out=m12[:, 4:5], in0=m12[:, 3:4], scalar1=1.0, scalar2=None, op0=ALU.add), reads=["m12"], writes=["m12"])
                S.op("dve", lambda e: e.reciprocal(out=m12[:, 5:6], in_=m12[:, 4:5]), reads=["m12"], writes=["m12"])
                S.op("dve", lambda e: e.tensor_tensor(out=m12[:, 6:7], in0=m12[:, 3:4], in1=m12[:, 5:6], op=ALU.mult), reads=["m12"], writes=["m12"])
                S.op("dve", lambda e, tt=tt: e.tensor_scalar(out=comb[:, tt, :], in0=comb[:, tt, :], scalar1=m12[:, 6:7], scalar2=None, op0=ALU.mult),
                     reads=["comb", "m12"], writes=["comb"])
                S.op("dve", lambda e, M1=M1, tt=tt: e.scalar_tensor_tensor(out=comb[:, tt, :], in0=M1, scalar=m12[:, 5:6], in1=comb[:, tt, :], op0=ALU.mult, op1=ALU.add),
                     reads=["tk", "m12", "comb"], writes=["comb"])
            di = 0
            for ex in range(8):
                for g in range(2):
                    b = bank_lo()
                    for t4 in range(4):
                        tt = g * 4 + t4
                        q = di % 2
                        di += 1
                        S.op("dve", lambda e, q=q, tt=tt, ex=ex: e.tensor_scalar(out=dg[q], in0=ident, scalar1=comb[:, tt, ex:ex + 1], scalar2=None, op0=ALU.mult),
                             reads=["ident", "comb"], writes=["dg%d" % q])
                        mm(b, 128, onesf, dg[q], True, True, ["onesf", "dg%d" % q], off=t4 * 128)
                    S.op("act", lambda e, b=b, g=g: e.activation(out=cb[:, g * 512:(g + 1) * 512], in_=ps[:, b, :], func=AF.Copy), reads=["ps%d" % b], writes=["cb"], acc=g > 0)
                ffn_pass(moe_g_d[ex], moe_u_d[ex], moe_d_d[ex], hbuf, wgu, wds, actT, sg, tmpu, cb)
            S.barrier()

        if "E" in phases:
            AR.seek(R_W)
            sq_t = AR.alloc([2, 512], BF16)
            rstd = AR.alloc([1024], F32)
            ob = [AR.alloc([16, 512], F32) for _ in range(2)]
            yv = y_d.rearrange("(c p) t -> p c t", p=128)
            for half in range(2):
                sl = slice(half * 512, (half + 1) * 512)
                b = bank_lo()
                for c in range(KC):
                    j = c % 2
                    S.op("act", lambda e, c=c, j=j, sl=sl: e.activation(out=sq_t[:, j, :], in_=xT[:, c, sl], func=AF.Square), reads=["xT%d" % c], writes=["sq%d" % j])
                    mm(b, 512, ones_bf[:], sq_t[:, j, :], c == 0, c == KC - 1, ["ones", "sq%d" % j], sig=True)
                S.op("act", lambda e, b=b, sl=sl: e.activation(out=rstd[:, sl], in_=ps[:, b, :], func=AF.Sqrt, bias=epst[:, 0:1], scale=1.0 / D),
                     reads=["ps%d" % b, "eps"], writes=["rstdE"])
                S.op("dve", lambda e, sl=sl: e.reciprocal(out=rstd[:, sl], in_=rstd[:, sl]), reads=["rstdE"], writes=["rstdE"])
                for c in range(KC):
                    S.op("dve", lambda e, c=c, sl=sl, half=half: e.scalar_tensor_tensor(out=ob[half][:, c, :], in0=xT[:, c, sl], scalar=gains[:, 3, c:c + 1],
                                                                                        in1=rstd[:, sl], op0=ALU.mult, op1=ALU.mult),
                         reads=["xT%d" % c, "rstdE", "gains"], writes=["ob%d" % half], acc=c > 0)
                S.dma("sp", yv[:, :, sl], ob[half], ["ob%d" % half], ["y"], acc=True)
        elif DEBUG == "attn":
            S.dma("pool", y_d.rearrange("(c p) t -> p c t", p=128), hbuf, [], ["y"])
        else:
            S.dma("sp", y_d.rearrange("(c p) t -> p c t", p=128), xT, ["xT%d" % m_ for m_ in range(16)], ["y"])
        S.barrier()

        with nc.Block() as block:
            @block.tensor
            def _(e):
                S.replay("pe", e)

            @block.scalar
            def _(e):
                S.replay("act", e)

            @block.vector
            def _(e):
                S.replay("dve", e)

            @block.gpsimd
            def _(e):
                S.replay("pool", e)

            @block.sync
            def _(e):
                S.replay("sp", e)
    return nc


def _pc(v):
    return np.ascontiguousarray(v.reshape(16, 128).T)


def _na_tables(rpb, j):
    rows = 64
    W = 64
    out = np.full((8, 4, 6, 128, 256), MASKVAL, np.float32)
    qc = np.arange(W)
    c_start = np.clip(qc - 8, 0, W - 16)
    for i in range(4):
        for s in range(6):
            for kr_l in range(2):
                kr = 16 * j - 4 + 4 * i + 2 * s + kr_l
                if kr < 0 or kr >= rows:
                    continue
                for qr_l in range(4):
                    qr = 16 * j + 4 * i + qr_l
                    rs = min(max(qr - 4, 0), rows - 8)
                    if not (rs <= kr < rs + 8):
                        continue
                    kcv = np.arange(W)
                    inwin = (kcv[:, None] >= c_start[None, :]) & (kcv[:, None] < c_start[None, :] + 16)
                    coff = np.clip(kcv[:, None] - qc[None, :], -15, 15) + 15
                    vals = rpb[:, kr - qr + 7, :][:, coff]
                    blk = out[:, i, s, kr_l * 64:(kr_l + 1) * 64, qr_l * 64:(qr_l + 1) * 64]
                    blk[:] = np.where(inwin[None], vals, MASKVAL)
    return np.ascontiguousarray(out.transpose(0, 1, 3, 2, 4))


def _rope_tables(pos):
    inv = 1.0 / (10000.0 ** (np.arange(0, 64, 2, dtype=np.float32) / 64))
    ang = pos.astype(np.float32)[:, None] * inv[None, :]
    cos, sin = np.cos(ang).T, np.sin(ang).T
    cosF = np.tile(cos, (4, 1))
    sinS = np.concatenate([-sin, sin, -sin, sin], 0)
    return np.ascontiguousarray(np.stack([cosF, sinS], 1).astype(np.float32))


def _prep(inp, phases):
    f = np.float32
    common = {}
    gains = np.stack([_pc(inp["ev_mix_norm"][0]), _pc(inp["ev_ffn_norm"][0]), _pc(inp["od_mix_norm"][0]), _pc(inp["final_norm"]),
                      _pc(inp["od_ffn_norm"][0])], 1)
    common["gains"] = np.ascontiguousarray(gains.astype(f))
    per = [dict() for _ in range(NCORES)]
    if "A" in phases:
        w_in = inp["ev_w_in"][0]
        common["w_in"] = w_in
        qk = w_in[:, 3072:5120].reshape(D, 2, 8, 2, 2, 32)
        common["w_sw"] = np.ascontiguousarray(qk[:, :, :, :, ::-1, :].reshape(D, 2048))
        common["w_out0"] = inp["ev_w_out"][0]
        common["ropek"] = _rope_tables(np.arange(4096))
        common["lamv"] = np.concatenate([inp["ev_da_lambda_q1"][0], inp["ev_da_lambda_k1"][0], inp["ev_da_lambda_q2"][0],
                                         inp["ev_da_lambda_k2"][0]])[None, :].astype(f)
        common["subln"] = inp["ev_da_subln"][0].reshape(128, 1).astype(f)
        xT_b = [np.ascontiguousarray(inp["x"][b].T) for b in range(2)]
        for core in range(NCORES):
            b, j = core // 4, core % 4
            per[core]["xf"] = xT_b[b]
            xh = np.zeros((D, 1536), f)
            lo, hi = 1024 * j - 256, 1024 * j + 1280
            slo, shi = max(lo, 0), min(hi, 4096)
            xh[:, slo - lo:shi - lo] = xT_b[b][:, slo:shi]
            per[core]["xh"] = xh
            per[core]["ropeq"] = _rope_tables(np.arange(1024 * j, 1024 * j + 1024))
            per[core]["nabias"] = _na_tables(inp["ev_na_rpb"][0], j)
    if "B" in phases:
        common["ffn_g"] = inp["ev_ffn_w_gate"][0]
        common["ffn_u"] = inp["ev_ffn_w_up"][0]
        common["ffn_d"] = inp["ev_ffn_w_down"][0]
    if "C" in phases:
        common["rw_in"] = inp["od_w_in"][0]
        common["rw_out"] = inp["od_w_out"][0]
        common["lruw"] = np.ascontiguousarray(np.stack([inp["od_lru_w_a"][0], inp["od_lru_w_x"][0]], 0))
        lrup = np.zeros((128, 5, 2, 16), f)
        for k, nm in enumerate(("od_lru_b_a", "od_lru_b_x", "od_lru_a_param")):
            for dr in range(2):
                lrup[:, k, dr, :] = _pc(inp[nm][0, dr])
        common["lrup"] = lrup
        convw = np.zeros((128, 16, 5), f)
        for k in range(4):
            convw[:, :, k] = _pc(inp["od_conv_w"][0, k])
        convw[:, :, 4] = _pc(inp["od_conv_b"][0])
        common["convw"] = convw
        for core in range(NCORES):
            b, j = core // 4, core % 4
            sel = np.zeros((128, 4, 8), f)
            if j > 0:
                sel[:, 0, core - 1] = 1.0
            if j < 3:
                sel[:, 1, core + 1] = 1.0
            for r in range(8):
                if r // 4 == b and r < core:
                    sel[:, 2, r] = 1.0
                if r // 4 == b and r > core:
                    sel[:, 3, r] = 1.0
            per[core]["sel"] = sel
    if "D" in phases:
        common["router"] = np.ascontiguousarray(inp["od_router"][0].reshape(16, 128, 8).transpose(1, 0, 2))
        common["ident"] = np.eye(128, dtype=f)
        common["moe_g"] = inp["od_moe_w_gate"][0]
        common["moe_u"] = inp["od_moe_w_up"][0]
        common["moe_d"] = inp["od_moe_w_down"][0]
    return common, per


_NC_CACHE = {}


def _run(phases, inp, x0T=None):
    if phases not in _NC_CACHE:
        _NC_CACHE[phases] = build(phases)
    nc = _NC_CACHE[phases]
    common, per = _prep(inp, phases)
    in_maps = []
    for core in range(NCORES):
        m = dict(common)
        m.update(per[core])
        if x0T is not None:
            m["x0T"] = x0T[core]
        in_maps.append(m)
    res = run_bass_kernel_spmd(nc, in_maps, core_ids=list(range(NCORES)))
    return [r["y"] for r in res.results]


def _assemble(ys):
    out = np.zeros((2, 4096, D), np.float32)
    for core in range(NCORES):
        b, j = core // 4, core % 4
        out[b, 1024 * j:1024 * (j + 1), :] = ys[core].T
    return out


PLAN = ["ABCDE"]


def kernel(**inputs):
    inp = {k: np.asarray(v) for k, v in inputs.items()}
    ys = None
    for ph in PLAN:
        ys = _run(ph, inp, ys)
    return _assemble(ys)
```

```python
import math
from contextlib import ExitStack
import numpy as np
import concourse.bass as bass
import concourse.mybir as mybir
from concourse.bass_utils import run_bass_kernel_spmd

F32 = mybir.dt.float32
BF16 = mybir.dt.bfloat16
AF = mybir.ActivationFunctionType
ALU = mybir.AluOpType
AX = mybir.AxisListType

NCORES = 8
T = 1024
D = 2048
KC = 16
DFF = 7168
EPS = 1e-6
LAMBDA_INIT = 0.8 - 0.6 * math.exp(0.0)
MASKVAL = -30000.0
DEBUG = ""


class Sched:
    ENG = ("pe", "act", "dve", "pool", "sp")

    def __init__(self, nc, stack):
        self.nc = nc
        self.stack = stack
        self.streams = {k: [] for k in self.ENG}
        self.sems = {}
        self.cnt = {}
        for k in ("pe", "act", "dve", "pool"):
            self.sems[k] = stack.enter_context(nc.semaphore("prog_" + k))
            self.cnt[k] = 0
        self.last_w = {}
        self.readers = {}
        self.waited = {k: {} for k in self.ENG}
        self.pe_pending = set()

    def _evs(self, reads, writes, acc):
        evs = []
        for k in reads:
            evs += list(self.last_w.get(k, {}).items())
        for k in writes:
            assert k not in self.pe_pending, k
            if not acc:
                evs += list(self.last_w.get(k, {}).items())
                evs += list(self.readers.get(k, {}).items())
        return evs

    def _filter(self, eng, evs):
        out = {}
        for sk, v in evs:
            if eng == "pe" and sk == "pe":
                continue
            if self.waited[eng].get(sk, 0) >= v:
                continue
            if out.get(sk, 0) < v:
                out[sk] = v
        for sk, v in out.items():
            self.waited[eng][sk] = v
        return list(out.items())

    def _record(self, ev, reads, writes, acc):
        for k in reads:
            d = self.readers.setdefault(k, {})
            d[ev[0]] = max(d.get(ev[0], 0), ev[1])
        for k in writes:
            if acc:
                d = self.last_w.setdefault(k, {})
                d[ev[0]] = max(d.get(ev[0], 0), ev[1])
            else:
                self.last_w[k] = {ev[0]: ev[1]}
                self.readers[k] = {}

    def op(self, eng, fn, reads=(), writes=(), acc=False, signal=True):
        waits = self._filter(eng, self._evs(reads, writes, acc))
        if signal:
            self.cnt[eng] += 1
            ev = (eng, self.cnt[eng])
            self.streams[eng].append((fn, waits, ev, 1))
            if eng == "pe" and self.pe_pending:
                self._record(ev, list(self.pe_pending), [], False)
                self.pe_pending = set()
            self._record(ev, reads, writes, acc)
            return ev
        assert eng == "pe"
        self.streams[eng].append((fn, waits, None, 0))
        self.pe_pending.update(reads)
        return None

    def dma(self, eng, out, in_, reads, writes, acc=False, **kw):
        waits = self._filter(eng, self._evs(reads, writes, acc))
        sk = "d_" + writes[0]
        if sk not in self.sems:
            self.sems[sk] = self.stack.enter_context(self.nc.semaphore(sk))
            self.cnt[sk] = 0
        self.cnt[sk] += 16
        ev = (sk, self.cnt[sk])
        def _dma(e):
            if getattr(self, "_cond", None) is not None:
                return e.dma_start(out=out, in_=in_, cond=self._cond, **kw)
            return e.dma_start(out=out, in_=in_, **kw)
        self.streams[eng].append((_dma, waits, ev, 16))
        self._record(ev, reads, writes, acc)
        return ev

    def cond_begin(self, cnt_ap, cnt_ev, thresh):
        assert not self.pe_pending
        self._snap = {}
        for eng in self.ENG:
            w = self._filter(eng, [cnt_ev])
            self.streams[eng].append(("IF", w, cnt_ap, thresh))
            self._snap[eng] = dict(self.waited[eng])

    def cond_end(self):
        assert not self.pe_pending
        for eng in self.ENG:
            self.streams[eng].append(("ENDIF", [], None, 0))
            self.waited[eng] = self._snap[eng]

    def barrier(self):
        assert not self.pe_pending
        evs = [(k, v) for k, v in self.cnt.items() if v > 0]
        for eng in self.ENG:
            w = self._filter(eng, evs)
            if w:
                self.streams[eng].append((None, w, None, 0))
        self.last_w.clear()
        self.readers.clear()

    def _emit(self, e, items):
        for fn, waits, ev, inc in items:
            for sk, v in waits:
                e.wait_ge(self.sems[sk], v)
            if fn is None:
                continue
            ins = fn(e)
            if ev is not None:
                ins.then_inc(self.sems[ev[0]], inc)

    def replay(self, eng, e):
        st = self.streams[eng]
        i = 0
        plain = []
        while i < len(st):
            it = st[i]
            if it[0] == "IF":
                self._emit(e, plain)
                plain = []
                j = i + 1
                while st[j][0] != "ENDIF":
                    j += 1
                body = st[i + 1:j]
                for sk, v in it[1]:
                    e.wait_ge(self.sems[sk], v)
                incs = {}
                for fn, waits, ev, inc in body:
                    if ev is not None:
                        incs[ev[0]] = incs.get(ev[0], 0) + inc
                if body and eng not in COND_ENGS:
                    if eng == "pool":
                        val = e.value_load(it[2], min_val=0, max_val=4096)
                        self._cond = val > it[3]
                    self._emit(e, body)
                    self._cond = None
                elif body:
                    self._rcn = getattr(self, "_rcn", 0) + 1
                    with e.register("rc%d" % self._rcn) as rc:
                        e.reg_load(rc, it[2])
                        CH = 96
                        for k0 in range(0, len(body), CH):
                            chunk = body[k0:k0 + CH]
                            cincs = {}
                            for fn_, waits_, ev_, inc_ in chunk:
                                if ev_ is not None:
                                    cincs[ev_[0]] = cincs.get(ev_[0], 0) + inc_
                            with e.If_lt(rc, it[3] + 1):
                                for sk, tot in cincs.items():
                                    e.sem_inc(self.sems[sk], tot)
                            with e.Else():
                                self._emit(e, chunk)
                i = j + 1
            else:
                plain.append(it)
                i += 1
        self._emit(e, plain)


class Arena:
    def __init__(self, t, nbytes):
        self.t = t
        self.n = nbytes
        self.off = 0

    def seek(self, off):
        self.off = off

    def alloc(self, shape, dt):
        esz = 4 if dt == F32 else 2
        n = int(np.prod(shape)) * esz
        n_al = (n + 63) // 64 * 64
        assert self.off + n_al <= self.n, (self.off, n_al, self.n)
        v = self.t[:, self.off // 2:(self.off + n) // 2]
        self.off += n_al
        if dt == F32:
            v = v.bitcast(F32)
        if len(shape) == 2:
            v = v.rearrange("p (a b) -> p a b", a=shape[0])
        elif len(shape) == 3:
            v = v.rearrange("p (a b c) -> p a b c", a=shape[0], b=shape[1])
        return v


R_X = 0
R_W = 64 * 1024
R_H = 168 * 1024
CAP = 384
NPASS = 3
TEST_NE = 8
COND_ENGS = ("pe", "dve", "pool")
TEST_NFG = 28
ARENA_BYTES = 200 * 1024


def build(phases="ABCDE"):
    nc = bass.Bass("TRN2", target_bir_lowering=False)
    first = phases[0]

    def din(name, shape, dt=F32):
        return nc.dram_tensor(name, list(shape), dt, kind="ExternalInput").ap()

    gains_d = din("gains", [128, 5, 16])
    if "A" in phases:
        xh_d = din("xh", [D, 1536])
        xf_d = din("xf", [D, 4096])
        w_in_d = din("w_in", [D, 6144])
        w_sw_d = din("w_sw", [D, 2048])
        w_out0_d = din("w_out0", [D, D])
        ropek_d = din("ropek", [128, 2, 4096])
        ropeq_d = din("ropeq", [128, 2, 1024])
        nab_d = din("nabias", [8, 4, 128, 6, 256])
        lamv_d = din("lamv", [1, 256])
        subln_d = din("subln", [128, 1])
        kscr = nc.dram_tensor("kscr", [8, 128, 4096], BF16).ap()
        vscr = nc.dram_tensor("vscr", [8, 128, 32, 128], BF16).ap()
    if first != "A":
        x0_d = din("x0T", [D, T])
    if "B" in phases:
        ffn_g_d = din("ffn_g", [D, DFF])
        ffn_u_d = din("ffn_u", [D, DFF])
        ffn_d_d = din("ffn_d", [DFF, D])
    if "C" in phases:
        rw_in_d = din("rw_in", [D, 4096])
        rw_out_d = din("rw_out", [D, D])
        lruw_d = din("lruw", [2, 2, 16, 128, 128])
        lrup_d = din("lrup", [128, 5, 2, 16])
        convw_d = din("convw", [128, 16, 5])
        sel_d = din("sel", [128, 4, 8])
        rscr = nc.dram_tensor("rscr", [16, 128, 1024], F32).ap()
        yscr = nc.dram_tensor("yscr", [16, 128, 1024], BF16).ap()
        ex1_in = nc.dram_tensor("ex1_in", [128, 48], F32)
        ex1_out = nc.dram_tensor("ex1_out", [NCORES * 128, 48], F32)
        ex2_in = nc.dram_tensor("ex2_in", [128, 64], F32)
        ex2_out = nc.dram_tensor("ex2_out", [NCORES * 128, 64], F32)
    if "S" in phases:
        router_d = din("router", [128, 16, 8])
        ident_d = din("ident", [128, 128])
        triu_d = din("triu", [128, 128])
        iota_d = din("iota", [128, CAP])
        sidx_d = din("sidx", [128, 9])
        grow_d = din("grow", [1, D])
        moe_g_d = din("moe_g", [TEST_NE, D, DFF])
        moe_u_d = din("moe_u", [TEST_NE, D, DFF])
        moe_d_d = din("moe_d", [TEST_NE, DFF, D])
        cnt_dram = nc.dram_tensor("cnt_dram", [1, 8], mybir.dt.int32).ap()
    if "D" in phases:
        router_d = din("router", [128, 16, 8])
        ident_d = din("ident", [128, 128])
        moe_g_d = din("moe_g", [8, D, DFF])
        moe_u_d = din("moe_u", [8, D, DFF])
        moe_d_d = din("moe_d", [8, DFF, D])
    y_d = nc.dram_tensor("y", [D, T], F32, kind="ExternalOutput").ap()

    with ExitStack() as stack:
        S = Sched(nc, stack)
        arena_t = stack.enter_context(nc.sbuf_tensor("arena", [128, ARENA_BYTES // 2], BF16))
        AR = Arena(arena_t, ARENA_BYTES)
        ps = stack.enter_context(nc.psum_tensor("ps", [128, 8, 512], F32))
        ones_bf = stack.enter_context(nc.sbuf_tensor("ones_bf", [128, 128], BF16))
        gains = stack.enter_context(nc.sbuf_tensor("gains_sb", [128, 5, 16], F32))
        epst = stack.enter_context(nc.sbuf_tensor("epst", [128, 1], F32))
        small = stack.enter_context(nc.sbuf_tensor("small", [128, 1344], F32))

        AR.seek(R_X)
        xT = AR.alloc([16, T], F32)
        AR.seek(R_H)
        hbuf = AR.alloc([16, T], BF16)

        S.op("dve", lambda e: e.memset(ones_bf[:], 1.0), writes=["ones"])
        S.op("dve", lambda e: e.memset(epst[:], EPS), writes=["eps"])
        S.dma("sp", gains[:], gains_d, [], ["gains"])

        rot = {"lo": 0, "hi": 0}

        def bank_lo():
            b = rot["lo"] % 4
            rot["lo"] += 1
            return b

        def bank_hi():
            b = 4 + rot["hi"] % 4
            rot["hi"] += 1
            return b

        def mm(bank, n, lhsT, rhs, start, stop, reads, off=0, sig=False):
            S.op("pe", lambda e: e.matmul(ps[:, bank, off:off + n], lhsT, rhs, start=start, stop=stop),
                 reads=reads, writes=["ps%d" % bank], acc=not start, signal=(stop or sig))

        def mm_part(bank, pr, n, lhsT, rhs, start, stop, reads, off=0):
            S.op("pe", lambda e: e.matmul(ps[pr[0]:pr[1], bank, off:off + n], lhsT, rhs, start=start, stop=stop),
                 reads=reads, writes=["ps%d" % bank], acc=not start, signal=stop)

        tmpc = {"i": 0}

        def rmsnorm(src, skey, n, gi, dst, dkey, sq_t, rstd, rkey, dacc=False):
            b = bank_lo()
            sk = (lambda c: skey % c) if "%d" in skey else (lambda c: skey)
            for c in range(KC):
                j = tmpc["i"] % 2
                tmpc["i"] += 1
                S.op("act", lambda e, c=c, j=j: e.activation(out=sq_t[:, j, 0:n], in_=src[:, c, 0:n], func=AF.Square),
                     reads=[sk(c)], writes=["sq%d" % j])
                mm(b, n, ones_bf[:], sq_t[:, j, 0:n], c == 0, c == KC - 1, ["ones", "sq%d" % j], sig=True)
            S.op("act", lambda e: e.activation(out=rstd[:, 0:n], in_=ps[:, b, 0:n], func=AF.Sqrt, bias=epst[:, 0:1], scale=1.0 / D),
                 reads=["ps%d" % b, "eps"], writes=[rkey])
            S.op("dve", lambda e: e.reciprocal(out=rstd[:, 0:n], in_=rstd[:, 0:n]), reads=[rkey], writes=[rkey])
            for c in range(KC):
                S.op("dve", lambda e, c=c: e.scalar_tensor_tensor(out=dst[:, c, 0:n], in0=src[:, c, 0:n], scalar=gains[:, gi, c:c + 1],
                                                                  in1=rstd[:, 0:n], op0=ALU.mult, op1=ALU.mult),
                     reads=[sk(c), rkey, "gains"], writes=[dkey], acc=(dacc or c > 0))

        def wview(w2d, c0, n):
            return w2d.rearrange("(c p) f -> p c f", p=128)[:, :, c0:c0 + n]

        if "A" in phases:
            def phase_A1():
                AR.seek(96 * 1024)
                wk = AR.alloc([16, 1024], BF16)
                wks = AR.alloc([16, 1024], BF16)
                wv = AR.alloc([16, 1024], BF16)
                AR.seek(R_X)
                xs = [AR.alloc([16, 256], F32) for _ in range(2)]
                hTa = [AR.alloc([16, 256], BF16) for _ in range(2)]
                kout = [AR.alloc([8, 256], BF16) for _ in range(2)]
                vout = [AR.alloc([2, 1024], BF16) for _ in range(2)]
                rk = [AR.alloc([2, 256], F32) for _ in range(2)]
                t1 = [AR.alloc([256], F32) for _ in range(2)]
                t2 = [AR.alloc([256], F32) for _ in range(2)]
                sq_t = AR.alloc([2, 512], BF16)
                rstd = [AR.alloc([256], F32) for _ in range(2)]
                for c in range(0, 16, 4):
                    S.dma("pool", wk[:, c:c + 4, :], wview(w_in_d, 4096, 1024)[:, c:c + 4, :], [], ["wk"], acc=c > 0)
                    S.dma("pool", wks[:, c:c + 4, :], wview(w_sw_d, 1024, 1024)[:, c:c + 4, :], [], ["wks"], acc=c > 0)
                    S.dma("pool", wv[:, c:c + 4, :], wview(w_in_d, 5120, 1024)[:, c:c + 4, :], [], ["wv"], acc=c > 0)
                ti = 0
                for tb in range(16):
                    j = tb % 2
                    S.dma("sp", xs[j], xf_d.rearrange("(c p) t -> p c t", p=128)[:, :, tb * 256:(tb + 1) * 256], [], ["xs%d" % j])
                    S.dma("sp", rk[j], ropek_d[:, :, tb * 256:(tb + 1) * 256], [], ["rk%d" % j])
                    rmsnorm(xs[j], "xs%d" % j, 256, 0, hTa[j], "hTa%d" % j, sq_t, rstd[j], "rstd%d" % j)
                    for h in range(8):
                        b1 = bank_lo()
                        for c in range(KC):
                            mm(b1, 256, wk[:, c, h * 128:(h + 1) * 128], hTa[j][:, c, :], c == 0, c == KC - 1, ["wk", "hTa%d" % j])
                        b2 = bank_lo()
                        for c in range(KC):
                            mm(b2, 256, wks[:, c, h * 128:(h + 1) * 128], hTa[j][:, c, :], c == 0, c == KC - 1, ["wks", "hTa%d" % j])
                        q = ti % 2
                        ti += 1
                        S.op("dve", lambda e, b1=b1, q=q, j=j: e.tensor_tensor(out=t1[q], in0=ps[:, b1, 0:256], in1=rk[j][:, 0, :], op=ALU.mult),
                             reads=["ps%d" % b1, "rk%d" % j], writes=["t1_%d" % q])
                        S.op("dve", lambda e, b2=b2, q=q, j=j: e.tensor_tensor(out=t2[q], in0=ps[:, b2, 0:256], in1=rk[j][:, 1, :], op=ALU.mult),
                             reads=["ps%d" % b2, "rk%d" % j], writes=["t2_%d" % q])
                        S.op("dve", lambda e, q=q, j=j, h=h: e.tensor_tensor(out=kout[j][:, h, :], in0=t1[q], in1=t2[q], op=ALU.add),
                             reads=["t1_%d" % q, "t2_%d" % q], writes=["kout%d" % j], acc=h > 0)
                    for tt in range(2):
                        for half in range(2):
                            b = bank_hi()
                            for c in range(KC):
                                mm(b, 512, hTa[j][:, c, tt * 128:(tt + 1) * 128], wv[:, c, half * 512:(half + 1) * 512], c == 0, c == KC - 1,
                                   ["wv", "hTa%d" % j])
                            S.op("act", lambda e, b=b, j=j, tt=tt, half=half: e.activation(out=vout[j][:, tt, half * 512:(half + 1) * 512],
                                                                                             in_=ps[:, b, :], func=AF.Copy),
                                 reads=["ps%d" % b], writes=["vout%d" % j], acc=(tt + half) > 0)
                    S.dma("sp", kscr.rearrange("h p t -> p h t")[:, :, tb * 256:(tb + 1) * 256], kout[j], ["kout%d" % j], ["kscr"], acc=True)
                    for tt in range(2):
                        S.dma("sp", vscr.rearrange("h p k d -> p k h d")[:, tb * 2 + tt, :, :],
                              vout[j][:, tt, :].rearrange("p (h d) -> p h d", h=8), ["vout%d" % j], ["vscr"], acc=True)
                S.barrier()

            phase_A1()
            def phase_A2():
                AR.seek(R_X)
                hTh = AR.alloc([16, 1536], BF16)
                base_a = AR.off
                xs2 = AR.alloc([16, 512], F32)
                sq_t = AR.alloc([2, 512], BF16)
                rstd2 = AR.alloc([512], F32)
                for (o, n) in ((0, 256), (256, 512), (768, 512), (1280, 256)):
                    S.dma("sp", xs2[:, :, 0:n], xh_d.rearrange("(c p) t -> p c t", p=128)[:, :, o:o + n], [], ["xs2"])
                    rmsnorm(xs2, "xs2", n, 0, hTh[:, :, o:o + n], "hTh", sq_t, rstd2, "rstd2", dacc=(o > 0))
                S.barrier()
                return hTh, base_a
            hTh, base_a = phase_A2()
            attnT = hbuf

            def phase_A3(hTh, base_a, attnT):
                AR.seek(base_a)
                wna = [AR.alloc([16, 3, 128], BF16) for _ in range(2)]
                qn = [AR.alloc([1024], BF16) for _ in range(2)]
                kn = [AR.alloc([1536], BF16) for _ in range(2)]
                vn = [AR.alloc([12, 128], BF16) for _ in range(2)]
                nb = [AR.alloc([6, 256], F32) for _ in range(2)]
                sb = [AR.alloc([256], F32) for _ in range(2)]
                pT = [AR.alloc([256], BF16) for _ in range(4)]
                rl = [AR.alloc([256], F32) for _ in range(2)]
                nbi = 0
                pi = 0
                for h in range(8):
                    j = h % 2
                    for w3, c0 in enumerate((h * 128, 1024 + h * 128, 2048 + h * 128)):
                        S.dma("pool", wna[j][:, :, w3, :], wview(w_in_d, c0, 128), [], ["wna%d" % j], acc=w3 > 0)
                    for half in range(2):
                        b = bank_lo()
                        for c in range(KC):
                            mm(b, 512, wna[j][:, c, 0, :], hTh[:, c, 256 + half * 512:256 + (half + 1) * 512], c == 0, c == KC - 1,
                               ["wna%d" % j, "hTh"])
                        S.op("act", lambda e, b=b, j=j, half=half: e.activation(out=qn[j][:, half * 512:(half + 1) * 512], in_=ps[:, b, :],
                                                                                 func=AF.Copy, scale=128.0 ** -0.5),
                             reads=["ps%d" % b], writes=["qn%d" % j], acc=half > 0)
                    for blk in range(3):
                        b = bank_lo()
                        for c in range(KC):
                            mm(b, 512, wna[j][:, c, 1, :], hTh[:, c, blk * 512:(blk + 1) * 512], c == 0, c == KC - 1, ["wna%d" % j, "hTh"])
                        S.op("act", lambda e, b=b, j=j, blk=blk: e.activation(out=kn[j][:, blk * 512:(blk + 1) * 512], in_=ps[:, b, :], func=AF.Copy),
                             reads=["ps%d" % b], writes=["kn%d" % j], acc=blk > 0)
                    for g in range(3):
                        b = bank_lo()
                        for t4 in range(4):
                            tt = g * 4 + t4
                            for c in range(KC):
                                mm(b, 128, hTh[:, c, tt * 128:(tt + 1) * 128], wna[j][:, c, 2, :], c == 0, c == KC - 1, ["wna%d" % j, "hTh"],
                                   off=t4 * 128)
                        S.op("act", lambda e, b=b, j=j, g=g: e.activation(out=vn[j][:, g * 4:(g + 1) * 4, :],
                                                                           in_=ps[:, b, :].rearrange("p (a b) -> p a b", a=4), func=AF.Copy),
                             reads=["ps%d" % b], writes=["vn%d" % j], acc=g > 0)
                    for i in range(4):
                        nj = nbi % 2
                        nbi += 1
                        S.dma("sp", nb[nj], nab_d[h, i], [], ["nb%d" % nj])
                        bo = bank_hi()
                        bl = bank_hi()
                        for s in range(6):
                            kc = 2 * i + s
                            bs = bank_lo()
                            mm(bs, 256, kn[j][:, kc * 128:(kc + 1) * 128], qn[j][:, i * 256:(i + 1) * 256], True, True, ["kn%d" % j, "qn%d" % j])
                            sj = pi % 2
                            pj = pi % 4
                            pi += 1
                            S.op("dve", lambda e, bs=bs, sj=sj, nj=nj, s=s: e.tensor_tensor(out=sb[sj], in0=ps[:, bs, 0:256], in1=nb[nj][:, s, :], op=ALU.add),
                                 reads=["ps%d" % bs, "nb%d" % nj], writes=["sb%d" % sj])
                            S.op("act", lambda e, sj=sj, pj=pj: e.activation(out=pT[pj], in_=sb[sj], func=AF.Exp),
                                 reads=["sb%d" % sj], writes=["pT%d" % pj])
                            mm(bo, 256, vn[j][:, kc, :], pT[pj], s == 0, s == 5, ["vn%d" % j, "pT%d" % pj])
                            mm(bl, 256, ones_bf[:], pT[pj], s == 0, s == 5, ["ones", "pT%d" % pj], sig=True)
                        rj = i % 2
                        S.op("dve", lambda e, bl=bl, rj=rj: e.reciprocal(out=rl[rj], in_=ps[:, bl, 0:256]), reads=["ps%d" % bl], writes=["rl%d" % rj])
                        S.op("dve", lambda e, bo=bo, rj=rj, h=h, i=i: e.tensor_tensor(out=attnT[:, h, i * 256:(i + 1) * 256], in0=ps[:, bo, 0:256],
                                                                                      in1=rl[rj], op=ALU.mult),
                             reads=["ps%d" % bo, "rl%d" % rj], writes=["attnT"], acc=True)
                S.barrier()

            phase_A3(hTh, base_a, attnT)
            def phase_A4(hTh, base_a, attnT):
                AR.seek(base_a)
                wda = [AR.alloc([16, 2, 128], BF16) for _ in range(2)]
                rq = AR.alloc([2, 1024], F32)
                qd = [AR.alloc([1024], BF16) for _ in range(2)]
                kd = [AR.alloc([4096], BF16) for _ in range(2)]
                vd = [AR.alloc([32, 128], BF16) for _ in range(2)]
                pT = [AR.alloc([512], BF16) for _ in range(4)]
                t1 = [AR.alloc([512], F32) for _ in range(2)]
                t2 = [AR.alloc([512], F32) for _ in range(2)]
                r0 = AR.alloc([512], F32)
                r1 = AR.alloc([512], F32)
                o0 = AR.alloc([512], F32)
                o1 = AR.alloc([512], F32)
                oo = AR.alloc([512], F32)
                sqd = AR.alloc([512], BF16)
                rsd = AR.alloc([512], F32)
                lam_t = AR.alloc([256], F32)
                lam_s = AR.alloc([8], F32)
                subl = AR.alloc([2], F32)
                S.dma("sp", rq, ropeq_d, [], ["rq"])
                S.dma("sp", lam_t, lamv_d.partition_broadcast(128).rearrange("p a b -> p (a b)"), [], ["lam_t"])
                S.dma("sp", subl[:, 0:1], subln_d, [], ["subl"])
                S.op("dve", lambda e: e.tensor_tensor(out=lam_t[:, 0:64], in0=lam_t[:, 0:64], in1=lam_t[:, 64:128], op=ALU.mult), reads=["lam_t"], writes=["lam_t"])
                S.op("dve", lambda e: e.tensor_tensor(out=lam_t[:, 128:192], in0=lam_t[:, 128:192], in1=lam_t[:, 192:256], op=ALU.mult), reads=["lam_t"], writes=["lam_t"])
                S.op("dve", lambda e: e.reduce_sum(out=lam_s[:, 0:1], in_=lam_t[:, 0:64], axis=AX.X), reads=["lam_t"], writes=["lam_s"])
                S.op("dve", lambda e: e.reduce_sum(out=lam_s[:, 1:2], in_=lam_t[:, 128:192], axis=AX.X), reads=["lam_t"], writes=["lam_s"])
                S.op("act", lambda e: e.activation(out=lam_s[:, 2:4], in_=lam_s[:, 0:2], func=AF.Exp), reads=["lam_s"], writes=["lam_s"])
                S.op("dve", lambda e: e.tensor_tensor(out=lam_s[:, 4:5], in0=lam_s[:, 3:4], in1=lam_s[:, 2:3], op=ALU.subtract), reads=["lam_s"], writes=["lam_s"])
                S.op("dve", lambda e: e.tensor_scalar(out=lam_s[:, 5:6], in0=lam_s[:, 4:5], scalar1=-LAMBDA_INIT, scalar2=None, op0=ALU.add), reads=["lam_s"], writes=["lam_s"])
                S.op("dve", lambda e: e.tensor_scalar(out=subl[:, 1:2], in0=subl[:, 0:1], scalar1=1.0 - LAMBDA_INIT, scalar2=None, op0=ALU.mult), reads=["subl"], writes=["subl"])
                pi = 0
                ti = 0
                for h in range(8):
                    j = h % 2
                    S.dma("pool", wda[j][:, :, 0, :], wview(w_in_d, 3072 + h * 128, 128), [], ["wda%d" % j])
                    S.dma("pool", wda[j][:, :, 1, :], wview(w_sw_d, h * 128, 128), [], ["wda%d" % j], acc=True)
                    S.dma("sp", kd[j], kscr[h], [], ["kd%d" % j])
                    S.dma("sp", vd[j], vscr[h], [], ["vd%d" % j])
                    for half in range(2):
                        b1 = bank_lo()
                        for c in range(KC):
                            mm(b1, 512, wda[j][:, c, 0, :], hTh[:, c, 256 + half * 512:256 + (half + 1) * 512], c == 0, c == KC - 1, ["wda%d" % j, "hTh"])
                        b2 = bank_lo()
                        for c in range(KC):
                            mm(b2, 512, wda[j][:, c, 1, :], hTh[:, c, 256 + half * 512:256 + (half + 1) * 512], c == 0, c == KC - 1, ["wda%d" % j, "hTh"])
                        q = ti % 2
                        ti += 1
                        S.op("dve", lambda e, b1=b1, q=q, half=half: e.scalar_tensor_tensor(out=t1[q], in0=ps[:, b1, :], scalar=0.125,
                                                                                            in1=rq[:, 0, half * 512:(half + 1) * 512], op0=ALU.mult, op1=ALU.mult),
                             reads=["ps%d" % b1, "rq"], writes=["t1_%d" % q])
                        S.op("dve", lambda e, b2=b2, q=q, half=half: e.scalar_tensor_tensor(out=t2[q], in0=ps[:, b2, :], scalar=0.125,
                                                                                            in1=rq[:, 1, half * 512:(half + 1) * 512], op0=ALU.mult, op1=ALU.mult),
                             reads=["ps%d" % b2, "rq"], writes=["t2_%d" % q])
                        S.op("dve", lambda e, q=q, j=j, half=half: e.tensor_tensor(out=qd[j][:, half * 512:(half + 1) * 512], in0=t1[q], in1=t2[q], op=ALU.add),
                             reads=["t1_%d" % q, "t2_%d" % q], writes=["qd%d" % j], acc=half > 0)
                    for qh in range(2):
                        steps = [(kc, c) for kc in range(32) for c in range(2)]
                        sbank = {}

                        def emit_s(idx):
                            kc, c = steps[idx]
                            bs = bank_lo()
                            sbank[idx] = bs
                            mm(bs, 512, kd[j][64 * c:64 * c + 64, kc * 128:(kc + 1) * 128], qd[j][64 * c:64 * c + 64, qh * 512:(qh + 1) * 512], True, True,
                               ["kd%d" % j, "qd%d" % j])

                        emit_s(0)
                        emit_s(1)
                        for idx in range(64):
                            kc, c = steps[idx]
                            bs = sbank[idx]
                            pj = pi % 4
                            pi += 1
                            S.op("act", lambda e, bs=bs, pj=pj: e.activation(out=pT[pj], in_=ps[:, bs, :], func=AF.Exp), reads=["ps%d" % bs], writes=["pT%d" % pj])
                            if idx + 2 < 64:
                                emit_s(idx + 2)
                            mm(4 + c, 512, vd[j][:, kc, :], pT[pj], kc == 0, kc == 31, ["vd%d" % j, "pT%d" % pj])
                            mm(6 + c, 512, ones_bf[:], pT[pj], kc == 0, kc == 31, ["ones", "pT%d" % pj], sig=True)
                        S.op("dve", lambda e: e.reciprocal(out=r0, in_=ps[:, 6, :]), reads=["ps6"], writes=["r0"])
                        S.op("dve", lambda e: e.reciprocal(out=r1, in_=ps[:, 7, :]), reads=["ps7"], writes=["r1"])
                        S.op("dve", lambda e: e.tensor_tensor(out=o0, in0=ps[:, 4, :], in1=r0, op=ALU.mult), reads=["ps4", "r0"], writes=["o0"])
                        S.op("dve", lambda e: e.tensor_tensor(out=o1, in0=ps[:, 5, :], in1=r1, op=ALU.mult), reads=["ps5", "r1"], writes=["o1"])
                        S.op("dve", lambda e: e.scalar_tensor_tensor(out=oo, in0=o1, scalar=lam_s[:, 5:6], in1=o0, op0=ALU.mult, op1=ALU.add),
                             reads=["o0", "o1", "lam_s"], writes=["oo"])
                        S.op("act", lambda e: e.activation(out=sqd, in_=oo, func=AF.Square), reads=["oo"], writes=["sqd"])
                        bn = bank_lo()
                        mm(bn, 512, ones_bf[:], sqd, True, True, ["ones", "sqd"])
                        S.op("act", lambda e, bn=bn: e.activation(out=rsd, in_=ps[:, bn, :], func=AF.Sqrt, bias=epst[:, 0:1], scale=1.0 / 128),
                             reads=["ps%d" % bn, "eps"], writes=["rsd"])
                        S.op("dve", lambda e: e.reciprocal(out=rsd, in_=rsd), reads=["rsd"], writes=["rsd"])
                        S.op("dve", lambda e, h=h, qh=qh: e.scalar_tensor_tensor(out=attnT[:, 8 + h, qh * 512:(qh + 1) * 512], in0=oo, scalar=subl[:, 1:2],
                                                                                 in1=rsd, op0=ALU.mult, op1=ALU.mult),
                             reads=["oo", "rsd", "subl"], writes=["attnT"], acc=True)
                S.barrier()

            phase_A4(hTh, base_a, attnT)
        if first == "A":
            S.dma("sp", xT, xh_d.rearrange("(c p) t -> p c t", p=128)[:, :, 256:1280], [], ["xT%d" % m_ for m_ in range(16)])
        else:
            S.dma("sp", xT, x0_d.rearrange("(c p) t -> p c t", p=128), [], ["xT%d" % m_ for m_ in range(16)])

        def out_proj(w_d, src, skey):
            AR.seek(R_W)
            wo = [AR.alloc([16, 512], BF16) for _ in range(2)]
            for mg in range(4):
                j = mg % 2
                S.dma("pool", wo[j], wview(w_d, mg * 512, 512), [], ["wo%d" % j])
                for m in range(4):
                    for half in range(2):
                        b = bank_hi()
                        for c in range(KC):
                            mm(b, 512, wo[j][:, c, m * 128:(m + 1) * 128], src[:, c, half * 512:(half + 1) * 512], c == 0, c == KC - 1, ["wo%d" % j, skey])
                        mt = mg * 4 + m
                        S.op("dve", lambda e, b=b, mt=mt, half=half: e.tensor_tensor(out=xT[:, mt, half * 512:(half + 1) * 512], in0=ps[:, b, :],
                                                                                       in1=xT[:, mt, half * 512:(half + 1) * 512], op=ALU.add),
                             reads=["ps%d" % b, "xT%d" % mt], writes=["xT%d" % mt])
            S.barrier()

        if "A" in phases and DEBUG != "attn":
            out_proj(w_out0_d, hbuf, "attnT")

        def ffn_pass(wg_d, wu_d, wd_d, hT, wgu, wds, actT, sg, tmpu, cb):
            st = {"n": 0}

            def load(fg):
                j = fg % 2
                S.dma("pool", wgu[j][:, :, 0, :], wview(wg_d, fg * 256, 256), [], ["wgu%d" % j])
                S.dma("pool", wgu[j][:, :, 1, :], wview(wu_d, fg * 256, 256), [], ["wgu%d" % j], acc=True)
                S.dma("pool", wds[j], wd_d[fg * 256:(fg + 1) * 256, :].rearrange("(j p) n -> p j n", p=128), [], ["wds%d" % j])

            def gu(fg):
                j = fg % 2
                for ft in range(2):
                    for th in range(2):
                        bg = bank_lo()
                        for c in range(KC):
                            mm(bg, 512, wgu[j][:, c, 0, ft * 128:(ft + 1) * 128], hT[:, c, th * 512:(th + 1) * 512], c == 0, c == KC - 1, ["wgu%d" % j, "hT"])
                        bu = bank_lo()
                        for c in range(KC):
                            mm(bu, 512, wgu[j][:, c, 1, ft * 128:(ft + 1) * 128], hT[:, c, th * 512:(th + 1) * 512], c == 0, c == KC - 1, ["wgu%d" % j, "hT"])
                        q = st["n"] % 2
                        st["n"] += 1
                        S.op("act", lambda e, bg=bg, q=q: e.activation(out=sg[q], in_=ps[:, bg, :], func=AF.Silu), reads=["ps%d" % bg], writes=["sg%d" % q])
                        akey = "act%d_%d" % (j, ft * 2 + th)
                        if cb is None:
                            S.op("dve", lambda e, bu=bu, q=q, j=j, ft=ft, th=th: e.tensor_tensor(out=actT[j][:, ft, th * 512:(th + 1) * 512], in0=ps[:, bu, :],
                                                                                                 in1=sg[q], op=ALU.mult),
                                 reads=["ps%d" % bu, "sg%d" % q], writes=[akey])
                        else:
                            S.op("dve", lambda e, bu=bu, q=q, th=th: e.tensor_tensor(out=tmpu[q], in0=ps[:, bu, :], in1=cb[:, th * 512:(th + 1) * 512], op=ALU.mult),
                                 reads=["ps%d" % bu, "cb"], writes=["tmpu%d" % q])
                            S.op("dve", lambda e, q=q, j=j, ft=ft, th=th: e.tensor_tensor(out=actT[j][:, ft, th * 512:(th + 1) * 512], in0=tmpu[q], in1=sg[q], op=ALU.mult),
                                 reads=["tmpu%d" % q, "sg%d" % q], writes=[akey])

            def down(fg):
                j = fg % 2
                for m in range(16):
                    for th in range(2):
                        b = bank_hi()
                        for ft in range(2):
                            mm(b, 512, wds[j][:, ft, m * 128:(m + 1) * 128], actT[j][:, ft, th * 512:(th + 1) * 512], ft == 0, ft == 1,
                               ["wds%d" % j, "act%d_%d" % (j, ft * 2 + th)])
                        S.op("dve", lambda e, b=b, m=m, th=th: e.tensor_tensor(out=xT[:, m, th * 512:(th + 1) * 512], in0=ps[:, b, :],
                                                                                 in1=xT[:, m, th * 512:(th + 1) * 512], op=ALU.add),
                             reads=["ps%d" % b, "xT%d" % m], writes=["xT%d" % m])

            NFG = DFF // 256
            load(0)
            load(1)
            gu(0)
            for fg in range(NFG):
                if fg + 1 < NFG:
                    gu(fg + 1)
                down(fg)
                if fg + 2 < NFG:
                    load(fg + 2)

        def ffn_bufs():
            AR.seek(R_W)
            wgu = [AR.alloc([16, 2, 256], BF16) for _ in range(2)]
            wds = [AR.alloc([2, 2048], BF16) for _ in range(2)]
            actT = [AR.alloc([2, 1024], BF16) for _ in range(2)]
            sg = [AR.alloc([512], F32) for _ in range(2)]
            tmpu = [AR.alloc([512], F32) for _ in range(2)]
            sq_t = AR.alloc([2, 512], BF16)
            rstd = AR.alloc([1024], F32)
            return wgu, wds, actT, sg, tmpu, sq_t, rstd

        def norm_x(gi, dst, dkey, sq_t, rstd):
            for half in range(2):
                rmsnorm(xT[:, :, half * 512:(half + 1) * 512], "xT%d", 512, gi, dst[:, :, half * 512:(half + 1) * 512], dkey, sq_t,
                        rstd[:, half * 512:(half + 1) * 512], "rstd", dacc=half > 0)

        if "B" in phases:
            wgu, wds, actT, sg, tmpu, sq_t, rstd = ffn_bufs()
            norm_x(1, hbuf, "hT", sq_t, rstd)
            ffn_pass(ffn_g_d, ffn_u_d, ffn_d_d, hbuf, wgu, wds, actT, sg, tmpu, None)
            S.barrier()

        if "C" in phases:
            AR.seek(R_W)
            edge = AR.alloc([16, 3], F32)
            g1 = AR.alloc([8, 48], F32)
            accL = AR.alloc([48], F32)
            accR = AR.alloc([48], F32)
            lrup = AR.alloc([5, 2, 16], F32)
            convw = AR.alloc([16, 5], F32)
            sel = AR.alloc([4, 8], F32)
            spv = AR.alloc([4, 32], F32)
            ex2 = AR.alloc([4, 16], F32)
            g2 = AR.alloc([8, 64], F32)
            car = AR.alloc([2, 16], F32)
            ctmp = AR.alloc([16], F32)
            sumga = AR.alloc([2, 16, 2], F32)
            base_c = AR.off
            wi = [AR.alloc([16, 512], BF16) for _ in range(2)]
            sq_t = AR.alloc([2, 512], BF16)
            rstd = AR.alloc([1024], F32)
            g1t = [AR.alloc([512], F32) for _ in range(2)]
            g2t = [AR.alloc([512], F32) for _ in range(2)]
            yt = [AR.alloc([512], BF16) for _ in range(2)]
            rt = [AR.alloc([512], F32) for _ in range(2)]
            S.dma("sp", lrup, lrup_d, [], ["lrup"])
            S.dma("sp", convw, convw_d, [], ["convw"])
            S.dma("sp", sel, sel_d, [], ["sel"])
            norm_x(2, hbuf, "hT", sq_t, rstd)
            lv = lrup[:, 2, :, :].rearrange("p a b -> p (a b)")
            S.op("dve", lambda e: e.tensor_scalar(out=spv[:, 0, :], in0=lv, scalar1=-1.0, scalar2=None, op0=ALU.mult), reads=["lrup"], writes=["spv"])
            S.op("dve", lambda e: e.tensor_tensor(out=spv[:, 1, :], in0=spv[:, 0, :], in1=lv, op=ALU.max), reads=["spv", "lrup"], writes=["spv"])
            S.op("act", lambda e: e.activation(out=spv[:, 2, :], in_=spv[:, 1, :], func=AF.Exp, scale=-1.0), reads=["spv"], writes=["spv"])
            S.op("act", lambda e: e.activation(out=spv[:, 2, :], in_=spv[:, 2, :], func=AF.Ln, bias=1.0), reads=["spv"], writes=["spv"])
            S.op("dve", lambda e: e.tensor_scalar(out=spv[:, 1, :], in0=spv[:, 0, :], scalar1=0.0, scalar2=None, op0=ALU.max), reads=["spv"], writes=["spv"])
            S.op("dve", lambda e: e.tensor_tensor(out=spv[:, 3, :], in0=spv[:, 1, :], in1=spv[:, 2, :], op=ALU.add), reads=["spv"], writes=["spv"])
            S.op("dve", lambda e: e.tensor_scalar(out=spv[:, 0, :], in0=spv[:, 3, :], scalar1=-8.0, scalar2=None, op0=ALU.mult), reads=["spv"], writes=["spv"])
            gi2 = 0
            for cg in range(8):
                j = cg % 2
                S.dma("pool", wi[j], wview(rw_in_d, cg * 512, 512), [], ["wi%d" % j])
                for m in range(4):
                    for half in range(2):
                        b = bank_lo()
                        for c in range(KC):
                            mm(b, 512, wi[j][:, c, m * 128:(m + 1) * 128], hbuf[:, c, half * 512:(half + 1) * 512], c == 0, c == KC - 1, ["wi%d" % j, "hT"])
                        q = gi2 % 2
                        gi2 += 1
                        if cg < 4:
                            n = cg * 4 + m
                            S.op("act", lambda e, b=b, q=q: e.activation(out=g1t[q], in_=ps[:, b, :], func=AF.Square), reads=["ps%d" % b], writes=["g1t%d" % q])
                            S.op("dve", lambda e, q=q: e.tensor_scalar(out=g1t[q], in0=g1t[q], scalar1=0.044715, scalar2=1.0, op0=ALU.mult, op1=ALU.add),
                                 reads=["g1t%d" % q], writes=["g1t%d" % q])
                            S.op("dve", lambda e, b=b, q=q: e.tensor_tensor(out=g1t[q], in0=ps[:, b, :], in1=g1t[q], op=ALU.mult),
                                 reads=["ps%d" % b, "g1t%d" % q], writes=["g1t%d" % q])
                            S.op("act", lambda e, q=q: e.activation(out=g2t[q], in_=g1t[q], func=AF.Sigmoid, scale=2.0 * math.sqrt(2.0 / math.pi)),
                                 reads=["g1t%d" % q], writes=["g2t%d" % q])
                            S.op("dve", lambda e, b=b, q=q: e.tensor_tensor(out=yt[q], in0=ps[:, b, :], in1=g2t[q], op=ALU.mult),
                                 reads=["ps%d" % b, "g2t%d" % q], writes=["yt%d" % q])
                            S.dma("sp", yscr[n][:, half * 512:(half + 1) * 512], yt[q], ["yt%d" % q], ["yscr"], acc=True)
                        else:
                            n = (cg - 4) * 4 + m
                            S.op("act", lambda e, b=b, q=q: e.activation(out=rt[q], in_=ps[:, b, :], func=AF.Copy), reads=["ps%d" % b], writes=["rt%d" % q])
                            if half == 0:
                                S.op("dve", lambda e, q=q, n=n: e.tensor_copy(out=edge[:, n, 0:2], in_=rt[q][:, 0:2]), reads=["rt%d" % q], writes=["edge"], acc=True)
                            else:
                                S.op("dve", lambda e, q=q, n=n: e.tensor_copy(out=edge[:, n, 2:3], in_=rt[q][:, 511:512]), reads=["rt%d" % q], writes=["edge"], acc=True)
                            S.dma("sp", rscr[n][:, half * 512:(half + 1) * 512], rt[q], ["rt%d" % q], ["rscr"], acc=True)
            S.dma("pool", ex1_in.ap(), edge.rearrange("p a b -> p (a b)"), ["edge"], ["ex1_in"])
            waits = S._filter("pool", S._evs(["ex1_in"], ["ex1_out"], False))
            S.sems["cc"] = stack.enter_context(nc.semaphore("cc"))
            S.cnt["cc"] = 1
            S.streams["pool"].append((lambda e: e.collective_compute("AllGather", ALU.bypass, replica_groups=[list(range(NCORES))],
                                                                     ins=[ex1_in.ap().opt()], outs=[ex1_out.ap().opt()]), waits, ("cc", 1), 1))
            S._record(("cc", 1), ["ex1_in"], ["ex1_out"], False)
            S.dma("pool", g1, ex1_out.ap().rearrange("(r p) f -> p r f", p=128), ["ex1_out"], ["g1"])
            for r in range(8):
                for acc_t, si, key in ((accL, 0, "accL"), (accR, 1, "accR")):
                    if r == 0:
                        S.op("dve", lambda e, acc_t=acc_t, si=si, r=r: e.tensor_scalar(out=acc_t, in0=g1[:, r, :], scalar1=sel[:, si, r:r + 1], scalar2=None, op0=ALU.mult),
                             reads=["g1", "sel"], writes=[key])
                    else:
                        S.op("dve", lambda e, acc_t=acc_t, si=si, r=r: e.scalar_tensor_tensor(out=acc_t, in0=g1[:, r, :], scalar=sel[:, si, r:r + 1], in1=acc_t,
                                                                                               op0=ALU.mult, op1=ALU.add),
                             reads=["g1", "sel", key], writes=[key])
            S.barrier()

            AR.seek(base_c)
            lw4 = AR.alloc([4, 16, 128], BF16)
            recx = AR.alloc([1032], F32)
            u = AR.alloc([1024], F32)
            ub = AR.alloc([1024], BF16)
            ga = AR.alloc([512], F32)
            gx = AR.alloc([512], F32)
            a_t = [AR.alloc([1024], F32) for _ in range(2)]
            b_t = [AR.alloc([1024], F32) for _ in range(2)]
            hs = [AR.alloc([1024], F32) for _ in range(2)]
            ybf = AR.alloc([1024], BF16)
            for ax in range(2):
                for dr in range(2):
                    S.dma("pool", lw4[:, ax * 2 + dr, :, :], lruw_d[ax, dr].rearrange("n i j -> i n j"), [], ["lw"], acc=(ax + dr) > 0)

            def lru_chunk(n, final):
                S.dma("sp", recx[:, 1:1025], rscr[n], [], ["recx"])
                S.op("dve", lambda e: e.tensor_copy(out=recx[:, 0:1], in_=accL[:, n * 3 + 2:n * 3 + 3]), reads=["accL"], writes=["recx"], acc=True)
                S.op("dve", lambda e: e.tensor_copy(out=recx[:, 1025:1027], in_=accR[:, n * 3:n * 3 + 2]), reads=["accR"], writes=["recx"], acc=True)
                S.op("dve", lambda e: e.tensor_scalar(out=u, in0=recx[:, 0:1024], scalar1=convw[:, n, 0:1], scalar2=convw[:, n, 4:5], op0=ALU.mult, op1=ALU.add),
                     reads=["recx", "convw"], writes=["u"])
                for k in range(1, 4):
                    S.op("dve", lambda e, k=k: e.scalar_tensor_tensor(out=u, in0=recx[:, k:k + 1024], scalar=convw[:, n, k:k + 1], in1=u, op0=ALU.mult, op1=ALU.add),
                         reads=["recx", "convw", "u"], writes=["u"])
                S.op("act", lambda e: e.activation(out=ub, in_=u, func=AF.Copy), reads=["u"], writes=["ub"])
                for dr in range(2):
                    for half in range(2):
                        sl = slice(half * 512, (half + 1) * 512)
                        ba = bank_lo()
                        mm(ba, 512, lw4[:, 0 * 2 + dr, n, :], ub[:, sl], True, True, ["lw", "ub"])
                        bx = bank_lo()
                        mm(bx, 512, lw4[:, 1 * 2 + dr, n, :], ub[:, sl], True, True, ["lw", "ub"])
                        S.op("act", lambda e, ba=ba, dr=dr, half=half: e.activation(out=ga, in_=ps[:, ba, :], func=AF.Sigmoid, bias=lrup[:, 0, dr, n:n + 1],
                                                                                    accum_out=sumga[:, dr, n, half:half + 1]),
                             reads=["ps%d" % ba, "lrup"], writes=["ga", "sumga"])
                        S.op("act", lambda e, bx=bx, dr=dr: e.activation(out=gx, in_=ps[:, bx, :], func=AF.Sigmoid, bias=lrup[:, 1, dr, n:n + 1]),
                             reads=["ps%d" % bx, "lrup"], writes=["gx"])
                        S.op("act", lambda e, dr=dr, sl=sl: e.activation(out=a_t[dr][:, sl], in_=ga, func=AF.Exp, scale=spv[:, 0, dr * 16 + n:dr * 16 + n + 1]),
                             reads=["ga", "spv"], writes=["a%d" % dr], acc=half > 0)
                        S.op("dve", lambda e, dr=dr, sl=sl: e.tensor_tensor(out=ga, in0=a_t[dr][:, sl], in1=a_t[dr][:, sl], op=ALU.mult),
                             reads=["a%d" % dr], writes=["ga"])
                        S.op("act", lambda e: e.activation(out=ga, in_=ga, func=AF.Sqrt, bias=1.0, scale=-1.0), reads=["ga"], writes=["ga"])
                        S.op("dve", lambda e, sl=sl: e.tensor_tensor(out=gx, in0=gx, in1=u[:, sl], op=ALU.mult), reads=["gx", "u"], writes=["gx"])
                        S.op("dve", lambda e, dr=dr, sl=sl: e.tensor_tensor(out=b_t[dr][:, sl], in0=gx, in1=ga, op=ALU.mult),
                             reads=["gx", "ga"], writes=["b%d" % dr], acc=half > 0)
                init_f = car[:, 0, n:n + 1] if final else 0.0
                init_b = car[:, 1, n:n + 1] if final else 0.0
                S.op("dve", lambda e: e.tensor_tensor_scan(out=hs[0], data0=a_t[0], data1=b_t[0], initial=init_f, op0=ALU.mult, op1=ALU.add),
                     reads=["a0", "b0", "car"], writes=["hs0"])
                S.op("dve", lambda e: e.tensor_tensor_scan(out=hs[1][:, ::-1], data0=a_t[1][:, ::-1], data1=b_t[1][:, ::-1], initial=init_b, op0=ALU.mult, op1=ALU.add),
                     reads=["a1", "b1", "car"], writes=["hs1"])
                if not final:
                    S.op("dve", lambda e: e.tensor_copy(out=ex2[:, 0, n:n + 1], in_=hs[0][:, 1023:1024]), reads=["hs0"], writes=["ex2"], acc=True)
                    S.op("dve", lambda e: e.tensor_copy(out=ex2[:, 2, n:n + 1], in_=hs[1][:, 0:1]), reads=["hs1"], writes=["ex2"], acc=True)
                else:
                    S.dma("sp", ybf, yscr[n], [], ["ybf"])
                    S.op("dve", lambda e: e.tensor_tensor(out=hs[0], in0=hs[0], in1=hs[1], op=ALU.add), reads=["hs0", "hs1"], writes=["hs0"])
                    S.op("dve", lambda e: e.tensor_tensor(out=hbuf[:, n, :], in0=hs[0], in1=ybf, op=ALU.mult), reads=["hs0", "ybf"], writes=["zT"], acc=True)

            for n in range(16):
                lru_chunk(n, False)
            for dr in range(2):
                S.op("dve", lambda e, dr=dr: e.tensor_tensor(out=ctmp, in0=sumga[:, dr, :, 0], in1=sumga[:, dr, :, 1], op=ALU.add), reads=["sumga"], writes=["ctmp"])
                S.op("dve", lambda e, dr=dr: e.tensor_tensor(out=ctmp, in0=ctmp, in1=spv[:, 0, dr * 16:(dr + 1) * 16], op=ALU.mult), reads=["ctmp", "spv"], writes=["ctmp"])
                S.op("act", lambda e, dr=dr: e.activation(out=ex2[:, 1 + 2 * dr, :], in_=ctmp, func=AF.Exp), reads=["ctmp"], writes=["ex2"], acc=True)
            S.dma("pool", ex2_in.ap(), ex2.rearrange("p a b -> p (a b)"), ["ex2"], ["ex2_in"])
            waits = S._filter("pool", S._evs(["ex2_in"], ["ex2_out"], False))
            S.cnt["cc"] = 2
            S.streams["pool"].append((lambda e: e.collective_compute("AllGather", ALU.bypass, replica_groups=[list(range(NCORES))],
                                                                     ins=[ex2_in.ap().opt()], outs=[ex2_out.ap().opt()]), waits, ("cc", 2), 1))
            S._record(("cc", 2), ["ex2_in"], ["ex2_out"], False)
            S.dma("pool", g2, ex2_out.ap().rearrange("(r p) f -> p r f", p=128), ["ex2_out"], ["g2"])
            S.op("dve", lambda e: e.memset(car.rearrange("p a b -> p (a b)"), 0.0), writes=["car"])
            for dr, order, mi in ((0, range(8), 2), (1, range(7, -1, -1), 3)):
                for r in order:
                    Hr = g2[:, r, dr * 32:dr * 32 + 16]
                    Ar = g2[:, r, dr * 32 + 16:dr * 32 + 32]
                    S.op("dve", lambda e, Ar=Ar, dr=dr: e.tensor_tensor(out=ctmp, in0=Ar, in1=car[:, dr, :], op=ALU.mult), reads=["g2", "car"], writes=["ctmp"])
                    S.op("dve", lambda e, Hr=Hr: e.tensor_tensor(out=ctmp, in0=ctmp, in1=Hr, op=ALU.add), reads=["g2", "ctmp"], writes=["ctmp"])
                    S.op("dve", lambda e, dr=dr: e.tensor_tensor(out=ctmp, in0=ctmp, in1=car[:, dr, :], op=ALU.subtract), reads=["car", "ctmp"], writes=["ctmp"])
                    S.op("dve", lambda e, dr=dr, r=r, mi=mi: e.scalar_tensor_tensor(out=car[:, dr, :], in0=ctmp, scalar=sel[:, mi, r:r + 1], in1=car[:, dr, :],
                                                                                    op0=ALU.mult, op1=ALU.add),
                         reads=["ctmp", "sel", "car"], writes=["car"])
            for n in range(16):
                lru_chunk(n, True)
            S.barrier()
            out_proj(rw_out_d, hbuf, "zT")

        if "D" in phases:
            wgu, wds, actT, sg, tmpu, sq_t, rstd = ffn_bufs()
            cb = AR.alloc([1024], F32)
            gr = AR.alloc([16, 8], F32)
            rt_sb = AR.alloc([16, 8], F32)
            ident = AR.alloc([128], F32)
            lg = AR.alloc([8, 8], F32)
            comb = AR.alloc([8, 8], F32)
            tk = AR.alloc([8, 8], F32)
            m12 = AR.alloc([16], F32)
            dg = [AR.alloc([128], F32) for _ in range(2)]
            rsT = AR.alloc([8, 8], F32)
            onesf = AR.alloc([128], F32)
            S.dma("sp", rt_sb, router_d, [], ["rt_sb"])
            S.dma("sp", ident, ident_d, [], ["ident"])
            S.op("dve", lambda e: e.memset(onesf, 1.0), writes=["onesf"])
            norm_x(4, hbuf, "hT", sq_t, rstd)
            for ex_ in range(8):
                S.op("dve", lambda e, ex_=ex_: e.tensor_tensor(out=gr[:, :, ex_], in0=rt_sb[:, :, ex_], in1=gains[:, 4, :], op=ALU.mult),
                     reads=["rt_sb", "gains"], writes=["gr"], acc=ex_ > 0)
            for tt in range(8):
                b = bank_lo()
                for c in range(KC):
                    mm(b, 8, xT[:, c, tt * 128:(tt + 1) * 128], gr[:, c, :], c == 0, c == KC - 1, ["xT%d" % c, "gr"])
                b2 = bank_lo()
                mm(b2, 8, rstd[0:1, tt * 128:(tt + 1) * 128], onesf[0:1, 0:8], True, True, ["rstd", "onesf"])
                S.op("act", lambda e, b2=b2, tt=tt: e.activation(out=rsT[:, tt, :], in_=ps[:, b2, 0:8], func=AF.Copy), reads=["ps%d" % b2], writes=["rsT"], acc=tt > 0)
                S.op("dve", lambda e, b=b, tt=tt: e.tensor_tensor(out=lg[:, tt, :], in0=ps[:, b, 0:8], in1=rsT[:, tt, :], op=ALU.mult),
                     reads=["ps%d" % b, "rsT"], writes=["lg"], acc=tt > 0)
            for tt in range(8):
                L = lg[:, tt, :]
                M1 = tk[:, tt, :]
                S.op("dve", lambda e, L=L: e.reduce_max(out=m12[:, 0:1], in_=L, axis=AX.X), reads=["lg"], writes=["m12"])
                S.op("dve", lambda e, L=L, M1=M1: e.tensor_scalar(out=M1, in0=L, scalar1=m12[:, 0:1], scalar2=None, op0=ALU.is_equal), reads=["lg", "m12"], writes=["tk"])
                S.op("dve", lambda e, L=L, M1=M1, tt=tt: e.scalar_tensor_tensor(out=comb[:, tt, :], in0=M1, scalar=-1e30, in1=L, op0=ALU.mult, op1=ALU.add),
                     reads=["tk", "lg"], writes=["comb"])
                S.op("dve", lambda e, tt=tt: e.reduce_max(out=m12[:, 1:2], in_=comb[:, tt, :], axis=AX.X), reads=["comb"], writes=["m12"])
                S.op("dve", lambda e, tt=tt: e.tensor_scalar(out=comb[:, tt, :], in0=comb[:, tt, :], scalar1=m12[:, 1:2], scalar2=None, op0=ALU.is_equal),
                     reads=["comb", "m12"], writes=["comb"])
                S.op("dve", lambda e: e.tensor_tensor(out=m12[:, 2:3], in0=m12[:, 1:2], in1=m12[:, 0:1], op=ALU.subtract), reads=["m12"], writes=["m12"])
                S.op("act", lambda e: e.activation(out=m12[:, 3:4], in_=m12[:, 2:3], func=AF.Exp), reads=["m12"], writes=["m12"])
                S.op("dve", lambda e: e.tensor_scalar(out=m12[:, 4:5], in0=m12[:, 3:4], scalar1=1.0, scalar2=None, op0=ALU.add), reads=["m12"], writes=["m12"])
                S.op("dve", lambda e: e.reciprocal(out=m12[:, 5:6], in_=m12[:, 4:5]), reads=["m12"], writes=["m12"])
                S.op("dve", lambda e: e.tensor_tensor(out=m12[:, 6:7], in0=m12[:, 3:4], in1=m12[:, 5:6], op=ALU.mult), reads=["m12"], writes=["m12"])
                S.op("dve", lambda e, tt=tt: e.tensor_scalar(out=comb[:, tt, :], in0=comb[:, tt, :], scalar1=m12[:, 6:7], scalar2=None, op0=ALU.mult),
                     reads=["comb", "m12"], writes=["comb"])
                S.op("dve", lambda e, M1=M1, tt=tt: e.scalar_tensor_tensor(out=comb[:, tt, :], in0=M1, scalar=m12[:, 5:6], in1=comb[:, tt, :], op0=ALU.mult, op1=ALU.add),
                     reads=["tk", "m12", "comb"], writes=["comb"])
            di = 0
            for ex in range(8):
                for g in range(2):
                    b = bank_lo()
                    for t4 in range(4):
                        tt = g * 4 + t4
                        q = di % 2
                        di += 1
                        S.op("dve", lambda e, q=q, tt=tt, ex=ex: e.tensor_scalar(out=dg[q], in0=ident, scalar1=comb[:, tt, ex:ex + 1], scalar2=None, op0=ALU.mult),
                             reads=["ident", "comb"], writes=["dg%d" % q])
                        mm(b, 128, onesf, dg[q], True, True, ["onesf", "dg%d" % q], off=t4 * 128)
                    S.op("act", lambda e, b=b, g=g: e.activation(out=cb[:, g * 512:(g + 1) * 512], in_=ps[:, b, :], func=AF.Copy), reads=["ps%d" % b], writes=["cb"], acc=g > 0)
                ffn_pass(moe_g_d[ex], moe_u_d[ex], moe_d_d[ex], hbuf, wgu, wds, actT, sg, tmpu, cb)
            S.barrier()


        if "S" in phases:
            def phase_S():
                I32 = mybir.dt.int32
                AR.seek(R_W)
                wgu = [AR.alloc([16, 2, 256], BF16) for _ in range(2)]
                wds = [AR.alloc([2, 2048], BF16) for _ in range(2)]
                hg = AR.alloc([16, CAP], BF16)
                Ysm = hg.rearrange("p a b -> p (a b)").rearrange("p (k f) -> p k f", k=3)
                Yacc = AR.alloc([16, CAP], F32)
                actT = [AR.alloc([2, CAP], BF16) for _ in range(2)]
                sg = [AR.alloc([CAP], F32) for _ in range(2)]
                G = AR.alloc([8, CAP], BF16)
                rhsS = G.rearrange("p a b -> p (a b)").rearrange("p (k h t) -> p k h t", k=3, h=2)
                posrow = AR.alloc([1024], F32)
                cb = AR.alloc([1024], F32)
                smallb = stack.enter_context(nc.sbuf_tensor("smallb", [128, 256], BF16))
                cnt_i = stack.enter_context(nc.sbuf_tensor("cnt_i", [128, 8], I32))
                trib = smallb[:, 0:128]
                mb = smallb[:, 128:192].rearrange("p (a b) -> p a b", a=8)
                ident = small[:, 0:128]
                iota = small[:, 128:512]
                dg = [small[:, 512:640], small[:, 640:768]]
                lg = small[:, 768:832].rearrange("p (a b) -> p a b", a=8)
                comb = small[:, 832:896].rearrange("p (a b) -> p a b", a=8)
                tk = small[:, 896:960].rearrange("p (a b) -> p a b", a=8)
                m12 = small[:, 960:976]
                pos = small[:, 976:1040].rearrange("p (a b) -> p a b", a=8)
                posp = small[:, 1040:1048]
                rs_tok = small[:, 1048:1056]
                ssq = small[:, 1056:1088].rearrange("p (a b) -> p a b", a=8)
                sidx = small[:, 1096:1105]
                gr = small[:, 512:640].rearrange("p (a b) -> p a b", a=16)
                rt_sb = small[:, 640:768].rearrange("p (a b) -> p a b", a=16)
                mf = small[:, 1105:1169].rearrange("p (a b) -> p a b", a=8)
                onesf = small[:, 1169:1297]
                S.op("dve", lambda e: e.memset(onesf, 1.0), writes=["onesf"])
                htok = hbuf.rearrange("p a b -> p (a b)").rearrange("p (t f) -> p t f", t=8)
                grow = Yacc.rearrange("p a b -> p (a b)")[:, 0:2048]
                junk = posrow[:, 0:512]
                S.dma("sp", rt_sb, router_d, [], ["rt_sb"])
                S.dma("sp", ident, ident_d, [], ["ident"])
                S.dma("sp", iota, iota_d, [], ["iota"])
                S.dma("sp", sidx, sidx_d, [], ["sidx"])
                S.dma("pool", trib, triu_d, [], ["trib"])
                S.dma("sp", grow, grow_d.partition_broadcast(128).rearrange("p a b -> p (a b)"), [], ["grow"])
                for pss in range(2):
                    for tt in range(8):
                        for cg in range(4):
                            b = bank_lo()
                            for c4 in range(4):
                                c = cg * 4 + c4
                                S.op("pe", lambda e, b=b, c4=c4, c=c, tt=tt: e.transpose(ps[:, b, c4 * 128:(c4 + 1) * 128], xT[:, c, tt * 128:(tt + 1) * 128], ident),
                                     reads=["xT%d" % c, "ident"], writes=["ps%d" % b], acc=c4 > 0, signal=(c4 == 3))
                            if pss == 0:
                                S.op("act", lambda e, b=b, tt=tt, cg=cg: e.activation(out=junk, in_=ps[:, b, :], func=AF.Square, accum_out=ssq[:, tt, cg:cg + 1]),
                                     reads=["ps%d" % b], writes=["junk", "ssq"])
                            else:
                                S.op("dve", lambda e, b=b, tt=tt, cg=cg: e.scalar_tensor_tensor(out=htok[:, tt, cg * 512:(cg + 1) * 512], in0=ps[:, b, :], scalar=rs_tok[:, tt:tt + 1],
                                                                                              in1=grow[:, cg * 512:(cg + 1) * 512], op0=ALU.mult, op1=ALU.mult),
                                     reads=["ps%d" % b, "rs_tok", "grow"], writes=["htok"], acc=(tt + cg) > 0)
                    if pss == 0:
                        S.op("dve", lambda e: e.reduce_sum(out=rs_tok, in_=ssq, axis=AX.X), reads=["ssq"], writes=["rs_tok"])
                        S.op("act", lambda e: e.activation(out=rs_tok, in_=rs_tok, func=AF.Sqrt, bias=epst[:, 0:1], scale=1.0 / D), reads=["rs_tok", "eps"], writes=["rs_tok"])
                        S.op("dve", lambda e: e.reciprocal(out=rs_tok, in_=rs_tok), reads=["rs_tok"], writes=["rs_tok"])
                for ex_ in range(8):
                    S.op("dve", lambda e, ex_=ex_: e.tensor_tensor(out=gr[:, :, ex_], in0=rt_sb[:, :, ex_], in1=gains[:, 4, :], op=ALU.mult),
                         reads=["rt_sb", "gains"], writes=["gr"], acc=ex_ > 0)
                for tt in range(8):
                    b = bank_lo()
                    for c in range(KC):
                        mm(b, 8, xT[:, c, tt * 128:(tt + 1) * 128], gr[:, c, :], c == 0, c == KC - 1, ["xT%d" % c, "gr"])
                    S.op("dve", lambda e, b=b, tt=tt: e.tensor_scalar(out=lg[:, tt, :], in0=ps[:, b, 0:8], scalar1=rs_tok[:, tt:tt + 1], scalar2=None, op0=ALU.mult),
                         reads=["ps%d" % b, "rs_tok"], writes=["lg"], acc=tt > 0)
                for tt in range(8):
                    L = lg[:, tt, :]
                    M1 = tk[:, tt, :]
                    S.op("dve", lambda e, L=L: e.reduce_max(out=m12[:, 0:1], in_=L, axis=AX.X), reads=["lg"], writes=["m12"])
                    S.op("dve", lambda e, L=L, M1=M1: e.tensor_scalar(out=M1, in0=L, scalar1=m12[:, 0:1], scalar2=None, op0=ALU.is_equal), reads=["lg", "m12"], writes=["tk"])
                    S.op("dve", lambda e, L=L, M1=M1, tt=tt: e.scalar_tensor_tensor(out=comb[:, tt, :], in0=M1, scalar=-1e30, in1=L, op0=ALU.mult, op1=ALU.add),
                         reads=["tk", "lg"], writes=["comb"])
                    S.op("dve", lambda e, tt=tt: e.reduce_max(out=m12[:, 1:2], in_=comb[:, tt, :], axis=AX.X), reads=["comb"], writes=["m12"])
                    S.op("dve", lambda e, tt=tt: e.tensor_scalar(out=comb[:, tt, :], in0=comb[:, tt, :], scalar1=m12[:, 1:2], scalar2=None, op0=ALU.is_equal),
                         reads=["comb", "m12"], writes=["comb"])
                    S.op("dve", lambda e: e.tensor_tensor(out=m12[:, 2:3], in0=m12[:, 1:2], in1=m12[:, 0:1], op=ALU.subtract), reads=["m12"], writes=["m12"])
                    S.op("act", lambda e: e.activation(out=m12[:, 3:4], in_=m12[:, 2:3], func=AF.Exp), reads=["m12"], writes=["m12"])
                    S.op("dve", lambda e: e.tensor_scalar(out=m12[:, 4:5], in0=m12[:, 3:4], scalar1=1.0, scalar2=None, op0=ALU.add), reads=["m12"], writes=["m12"])
                    S.op("dve", lambda e: e.reciprocal(out=m12[:, 5:6], in_=m12[:, 4:5]), reads=["m12"], writes=["m12"])
                    S.op("dve", lambda e: e.tensor_tensor(out=m12[:, 6:7], in0=m12[:, 3:4], in1=m12[:, 5:6], op=ALU.mult), reads=["m12"], writes=["m12"])
                    S.op("dve", lambda e, M1=M1, tt=tt: e.tensor_tensor(out=mf[:, tt, :], in0=M1, in1=comb[:, tt, :], op=ALU.add), reads=["tk", "comb"], writes=["mf"], acc=tt > 0)
                    S.op("dve", lambda e, tt=tt: e.tensor_scalar(out=comb[:, tt, :], in0=comb[:, tt, :], scalar1=m12[:, 6:7], scalar2=None, op0=ALU.mult),
                         reads=["comb", "m12"], writes=["comb"])
                    S.op("dve", lambda e, M1=M1, tt=tt: e.scalar_tensor_tensor(out=comb[:, tt, :], in0=M1, scalar=m12[:, 5:6], in1=comb[:, tt, :], op0=ALU.mult, op1=ALU.add),
                         reads=["tk", "m12", "comb"], writes=["comb"])
                S.op("dve", lambda e: e.tensor_copy(out=mb, in_=mf), reads=["mf"], writes=["mb"])
                b = bank_lo()
                for tt in range(8):
                    mm(b, 8, ones_bf[:], mb[:, tt, :], tt == 0, tt == 7, ["ones", "mb"])
                S.op("dve", lambda e, b=b: e.tensor_copy(out=cnt_i[0:1, :], in_=ps[0:1, b, 0:8]), reads=["ps%d" % b], writes=["cnt_i"])
                cnt_ev = S.dma("sp", cnt_dram, cnt_i[0:1, :], ["cnt_i"], ["cnt_dram"])
                for tt in range(8):
                    b = bank_lo()
                    mm(b, 8, trib, mb[:, tt, :], True, tt == 0, ["trib", "mb"])
                    for t2 in range(tt):
                        mm(b, 8, ones_bf[:], mb[:, t2, :], False, t2 == tt - 1, ["ones", "mb"])
                    S.op("dve", lambda e, b=b, tt=tt: e.tensor_copy(out=pos[:, tt, :], in_=ps[:, b, 0:8]), reads=["ps%d" % b], writes=["pos"], acc=tt > 0)
                S.barrier()

                def bcast_row(dst, dkey, src3, ex):
                    for g in range(2):
                        bb = bank_lo()
                        for t4 in range(4):
                            tt = g * 4 + t4
                            q = (g * 4 + t4) % 2
                            S.op("dve", lambda e, q=q, tt=tt: e.tensor_scalar(out=dg[q], in0=ident, scalar1=src3[:, tt, ex:ex + 1], scalar2=None, op0=ALU.mult),
                                 reads=["ident", "comb", "pos"], writes=["dg%d" % q])
                            mm(bb, 128, onesf, dg[q], True, True, ["onesf", "dg%d" % q], off=t4 * 128)
                        S.op("act", lambda e, bb=bb, g=g: e.activation(out=dst[:, g * 512:(g + 1) * 512], in_=ps[:, bb, :], func=AF.Copy), reads=["ps%d" % bb], writes=[dkey], acc=g > 0)


                def sparse_ffn(wg_d, wu_d, wd_d):
                    st = {"n": 0}

                    def load(fg):
                        j = fg % 2
                        S.dma("pool", wgu[j][:, :, 0, :], wview(wg_d, fg * 256, 256), [], ["wgu%d" % j])
                        S.dma("pool", wgu[j][:, :, 1, :], wview(wu_d, fg * 256, 256), [], ["wgu%d" % j], acc=True)
                        S.dma("pool", wds[j], wd_d[fg * 256:(fg + 1) * 256, :].rearrange("(j p) n -> p j n", p=128), [], ["wds%d" % j])

                    def gu(fg):
                        j = fg % 2
                        for ft in range(2):
                            bg = bank_lo()
                            for c in range(KC):
                                mm(bg, CAP, wgu[j][:, c, 0, ft * 128:(ft + 1) * 128], hg[:, c, :], c == 0, c == KC - 1, ["wgu%d" % j, "hgY"])
                            bu = bank_lo()
                            for c in range(KC):
                                mm(bu, CAP, wgu[j][:, c, 1, ft * 128:(ft + 1) * 128], hg[:, c, :], c == 0, c == KC - 1, ["wgu%d" % j, "hgY"])
                            q = st["n"] % 2
                            st["n"] += 1
                            S.op("act", lambda e, bg=bg, q=q: e.activation(out=sg[q], in_=ps[:, bg, 0:CAP], func=AF.Silu), reads=["ps%d" % bg], writes=["sg%d" % q])
                            S.op("dve", lambda e, bu=bu, q=q, j=j, ft=ft: e.tensor_tensor(out=actT[j][:, ft, :], in0=ps[:, bu, 0:CAP], in1=sg[q], op=ALU.mult),
                                 reads=["ps%d" % bu, "sg%d" % q], writes=["act%d_%d" % (j, ft)])

                    def down(fg):
                        j = fg % 2
                        for m in range(16):
                            b = bank_hi()
                            for ft in range(2):
                                mm(b, CAP, wds[j][:, ft, m * 128:(m + 1) * 128], actT[j][:, ft, :], ft == 0, ft == 1, ["wds%d" % j, "act%d_%d" % (j, ft)])
                            if fg == 0:
                                S.op("dve", lambda e, b=b, m=m: e.tensor_copy(out=Yacc[:, m, :], in_=ps[:, b, 0:CAP]), reads=["ps%d" % b], writes=["Yacc%d" % m])
                            else:
                                S.op("dve", lambda e, b=b, m=m: e.tensor_tensor(out=Yacc[:, m, :], in0=ps[:, b, 0:CAP], in1=Yacc[:, m, :], op=ALU.add),
                                     reads=["ps%d" % b, "Yacc%d" % m], writes=["Yacc%d" % m])

                    NFG = TEST_NFG
                    load(0)
                    load(1)
                    gu(0)
                    for fg in range(NFG):
                        if fg + 1 < NFG:
                            gu(fg + 1)
                        down(fg)
                        if fg + 2 < NFG:
                            load(fg + 2)

                for ex in range(TEST_NE):
                    bcast_row(cb, "cb", comb, ex)
                    bcast_row(posrow, "posrow", pos, ex)
                    for p in range(NPASS):
                        if p > 0:
                            S.cond_begin(cnt_dram[0:1, ex:ex + 1], cnt_ev, CAP * p)
                        S.op("dve", lambda e, ex=ex, p=p: e.tensor_scalar(out=posp, in0=pos[:, :, ex], scalar1=-float(CAP * p), scalar2=None, op0=ALU.add),
                             reads=["pos"], writes=["posp"])
                        for tt in range(8):
                            S.op("dve", lambda e, tt=tt, ex=ex: e.tensor_scalar(out=G[:, tt, :], in0=iota, scalar1=posp[:, tt:tt + 1], scalar2=mf[:, tt, ex:ex + 1],
                                                                                 op0=ALU.is_equal, op1=ALU.mult),
                                 reads=["iota", "posp", "mf"], writes=["Grs"], acc=tt > 0)
                        for c in range(KC):
                            b = bank_lo()
                            for tt in range(8):
                                mm(b, CAP, htok[:, tt, c * 128:(c + 1) * 128], G[:, tt, :], tt == 0, tt == 7, ["htok", "Grs"])
                            S.op("act", lambda e, b=b, c=c: e.activation(out=hg[:, c, :], in_=ps[:, b, 0:CAP], func=AF.Copy), reads=["ps%d" % b], writes=["hgY"], acc=c > 0)
                        sparse_ffn(moe_g_d[ex], moe_u_d[ex], moe_d_d[ex])
                        for k in range(3):
                            for mg in range(4):
                                b = bank_lo()
                                for m4 in range(4):
                                    m = mg * 4 + m4
                                    S.op("pe", lambda e, b=b, m4=m4, m=m, k=k: e.transpose(ps[:, b, m4 * 128:(m4 + 1) * 128], Yacc[:, m, k * 128:(k + 1) * 128], ident),
                                         reads=["Yacc%d" % m, "ident"], writes=["ps%d" % b], acc=m4 > 0, signal=(m4 == 3))
                                S.op("act", lambda e, b=b, k=k, mg=mg: e.activation(out=Ysm[:, k, mg * 512:(mg + 1) * 512], in_=ps[:, b, :], func=AF.Copy),
                                     reads=["ps%d" % b], writes=["hgY"], acc=(k + mg) > 0)
                        for k in range(3):
                            for half in range(2):
                                S.op("dve", lambda e, k=k, half=half, p=p: e.scalar_tensor_tensor(out=rhsS[:, k, half, :], in0=posrow[:, half * 512:(half + 1) * 512],
                                                                                                 scalar=sidx[:, p * 3 + k:p * 3 + k + 1], in1=cb[:, half * 512:(half + 1) * 512],
                                                                                                 op0=ALU.is_equal, op1=ALU.mult),
                                     reads=["posrow", "sidx", "cb"], writes=["Grs"], acc=(k + half) > 0)
                        for m in range(16):
                            for half in range(2):
                                b = bank_hi()
                                for k in range(3):
                                    mm(b, 512, Ysm[:, k, m * 128:(m + 1) * 128], rhsS[:, k, half, :], k == 0, k == 2, ["hgY", "Grs"])
                                S.op("dve", lambda e, b=b, m=m, half=half: e.tensor_tensor(out=xT[:, m, half * 512:(half + 1) * 512], in0=ps[:, b, :],
                                                                                           in1=xT[:, m, half * 512:(half + 1) * 512], op=ALU.add),
                                     reads=["ps%d" % b, "xT%d" % m], writes=["xT%d" % m])
                        if p > 0:
                            S.cond_end()
                S.barrier()

            phase_S()

        if "E" in phases:
            AR.seek(R_W)
            sq_t = AR.alloc([2, 512], BF16)
            rstd = AR.alloc([1024], F32)
            ob = [AR.alloc([16, 512], F32) for _ in range(2)]
            yv = y_d.rearrange("(c p) t -> p c t", p=128)
            for half in range(2):
                sl = slice(half * 512, (half + 1) * 512)
                b = bank_lo()
                for c in range(KC):
                    j = c % 2
                    S.op("act", lambda e, c=c, j=j, sl=sl: e.activation(out=sq_t[:, j, :], in_=xT[:, c, sl], func=AF.Square), reads=["xT%d" % c], writes=["sq%d" % j])
                    mm(b, 512, ones_bf[:], sq_t[:, j, :], c == 0, c == KC - 1, ["ones", "sq%d" % j], sig=True)
                S.op("act", lambda e, b=b, sl=sl: e.activation(out=rstd[:, sl], in_=ps[:, b, :], func=AF.Sqrt, bias=epst[:, 0:1], scale=1.0 / D),
                     reads=["ps%d" % b, "eps"], writes=["rstdE"])
                S.op("dve", lambda e, sl=sl: e.reciprocal(out=rstd[:, sl], in_=rstd[:, sl]), reads=["rstdE"], writes=["rstdE"])
                for c in range(KC):
                    S.op("dve", lambda e, c=c, sl=sl, half=half: e.scalar_tensor_tensor(out=ob[half][:, c, :], in0=xT[:, c, sl], scalar=gains[:, 3, c:c + 1],
                                                                                        in1=rstd[:, sl], op0=ALU.mult, op1=ALU.mult),
                         reads=["xT%d" % c, "rstdE", "gains"], writes=["ob%d" % half], acc=c > 0)
                S.dma("sp", yv[:, :, sl], ob[half], ["ob%d" % half], ["y"], acc=True)
        elif DEBUG == "attn":
            S.dma("pool", y_d.rearrange("(c p) t -> p c t", p=128), hbuf, [], ["y"])
        else:
            S.dma("sp", y_d.rearrange("(c p) t -> p c t", p=128), xT, ["xT%d" % m_ for m_ in range(16)], ["y"])
        S.barrier()

        with nc.Block() as block:
            @block.tensor
            def _(e):
                S.replay("pe", e)

            @block.scalar
            def _(e):
                S.replay("act", e)

            @block.vector
            def _(e):
                S.replay("dve", e)

            @block.gpsimd
            def _(e):
                S.replay("pool", e)

            @block.sync
            def _(e):
                S.replay("sp", e)
    return nc


def _pc(v):
    return np.ascontiguousarray(v.reshape(16, 128).T)


def _na_tables(rpb, j):
    rows = 64
    W = 64
    out = np.full((8, 4, 6, 128, 256), MASKVAL, np.float32)
    qc = np.arange(W)
    c_start = np.clip(qc - 8, 0, W - 16)
    for i in range(4):
        for s in range(6):
            for kr_l in range(2):
                kr = 16 * j - 4 + 4 * i + 2 * s + kr_l
                if kr < 0 or kr >= rows:
                    continue
                for qr_l in range(4):
                    qr = 16 * j + 4 * i + qr_l
                    rs = min(max(qr - 4, 0), rows - 8)
                    if not (rs <= kr < rs + 8):
                        continue
                    kcv = np.arange(W)
                    inwin = (kcv[:, None] >= c_start[None, :]) & (kcv[:, None] < c_start[None, :] + 16)
                    coff = np.clip(kcv[:, None] - qc[None, :], -15, 15) + 15
                    vals = rpb[:, kr - qr + 7, :][:, coff]
                    blk = out[:, i, s, kr_l * 64:(kr_l + 1) * 64, qr_l * 64:(qr_l + 1) * 64]
                    blk[:] = np.where(inwin[None], vals, MASKVAL)
    return np.ascontiguousarray(out.transpose(0, 1, 3, 2, 4))


def _rope_tables(pos):
    inv = 1.0 / (10000.0 ** (np.arange(0, 64, 2, dtype=np.float32) / 64))
    ang = pos.astype(np.float32)[:, None] * inv[None, :]
    cos, sin = np.cos(ang).T, np.sin(ang).T
    cosF = np.tile(cos, (4, 1))
    sinS = np.concatenate([-sin, sin, -sin, sin], 0)
    return np.ascontiguousarray(np.stack([cosF, sinS], 1).astype(np.float32))


def _prep(inp, phases):
    f = np.float32
    common = {}
    gains = np.stack([_pc(inp["ev_mix_norm"][0]), _pc(inp["ev_ffn_norm"][0]), _pc(inp["od_mix_norm"][0]), _pc(inp["final_norm"]),
                      _pc(inp["od_ffn_norm"][0])], 1)
    common["gains"] = np.ascontiguousarray(gains.astype(f))
    per = [dict() for _ in range(NCORES)]
    if "A" in phases:
        w_in = inp["ev_w_in"][0]
        common["w_in"] = w_in
        qk = w_in[:, 3072:5120].reshape(D, 2, 8, 2, 2, 32)
        common["w_sw"] = np.ascontiguousarray(qk[:, :, :, :, ::-1, :].reshape(D, 2048))
        common["w_out0"] = inp["ev_w_out"][0]
        common["ropek"] = _rope_tables(np.arange(4096))
        common["lamv"] = np.concatenate([inp["ev_da_lambda_q1"][0], inp["ev_da_lambda_k1"][0], inp["ev_da_lambda_q2"][0],
                                         inp["ev_da_lambda_k2"][0]])[None, :].astype(f)
        common["subln"] = inp["ev_da_subln"][0].reshape(128, 1).astype(f)
        xT_b = [np.ascontiguousarray(inp["x"][b].T) for b in range(2)]
        for core in range(NCORES):
            b, j = core // 4, core % 4
            per[core]["xf"] = xT_b[b]
            xh = np.zeros((D, 1536), f)
            lo, hi = 1024 * j - 256, 1024 * j + 1280
            slo, shi = max(lo, 0), min(hi, 4096)
            xh[:, slo - lo:shi - lo] = xT_b[b][:, slo:shi]
            per[core]["xh"] = xh
            per[core]["ropeq"] = _rope_tables(np.arange(1024 * j, 1024 * j + 1024))
            per[core]["nabias"] = _na_tables(inp["ev_na_rpb"][0], j)
    if "B" in phases:
        common["ffn_g"] = inp["ev_ffn_w_gate"][0]
        common["ffn_u"] = inp["ev_ffn_w_up"][0]
        common["ffn_d"] = inp["ev_ffn_w_down"][0]
    if "C" in phases:
        common["rw_in"] = inp["od_w_in"][0]
        common["rw_out"] = inp["od_w_out"][0]
        common["lruw"] = np.ascontiguousarray(np.stack([inp["od_lru_w_a"][0], inp["od_lru_w_x"][0]], 0))
        lrup = np.zeros((128, 5, 2, 16), f)
        for k, nm in enumerate(("od_lru_b_a", "od_lru_b_x", "od_lru_a_param")):
            for dr in range(2):
                lrup[:, k, dr, :] = _pc(inp[nm][0, dr])
        common["lrup"] = lrup
        convw = np.zeros((128, 16, 5), f)
        for k in range(4):
            convw[:, :, k] = _pc(inp["od_conv_w"][0, k])
        convw[:, :, 4] = _pc(inp["od_conv_b"][0])
        common["convw"] = convw
        for core in range(NCORES):
            b, j = core // 4, core % 4
            sel = np.zeros((128, 4, 8), f)
            if j > 0:
                sel[:, 0, core - 1] = 1.0
            if j < 3:
                sel[:, 1, core + 1] = 1.0
            for r in range(8):
                if r // 4 == b and r < core:
                    sel[:, 2, r] = 1.0
                if r // 4 == b and r > core:
                    sel[:, 3, r] = 1.0
            per[core]["sel"] = sel
    if "S" in phases:
        common["router"] = np.ascontiguousarray(inp["od_router"][0].reshape(16, 128, 8).transpose(1, 0, 2))
        common["ident"] = np.eye(128, dtype=f)
        common["triu"] = np.triu(np.ones((128, 128), f), 1)
        common["iota"] = np.ascontiguousarray(np.tile(np.arange(CAP, dtype=f)[None, :], (128, 1)))
        common["sidx"] = np.ascontiguousarray((np.arange(128, dtype=f)[:, None] + 128.0 * np.arange(9, dtype=f)[None, :]).astype(f))
        common["grow"] = inp["od_ffn_norm"][0].reshape(1, D).astype(f)
        common["moe_g"] = inp["od_moe_w_gate"][0][:TEST_NE]
        common["moe_u"] = inp["od_moe_w_up"][0][:TEST_NE]
        common["moe_d"] = inp["od_moe_w_down"][0][:TEST_NE]
    if "D" in phases:
        common["router"] = np.ascontiguousarray(inp["od_router"][0].reshape(16, 128, 8).transpose(1, 0, 2))
        common["ident"] = np.eye(128, dtype=f)
        common["moe_g"] = inp["od_moe_w_gate"][0]
        common["moe_u"] = inp["od_moe_w_up"][0]
        common["moe_d"] = inp["od_moe_w_down"][0]
    return common, per


_NC_CACHE = {}


def _run(phases, inp, x0T=None):
    if phases not in _NC_CACHE:
        _NC_CACHE[phases] = build(phases)
    nc = _NC_CACHE[phases]
    common, per = _prep(inp, phases)
    in_maps = []
    for core in range(NCORES):
        m = dict(common)
        m.update(per[core])
        if x0T is not None:
            m["x0T"] = x0T[core]
        in_maps.append(m)
    res = run_bass_kernel_spmd(nc, in_maps, core_ids=list(range(NCORES)))
    return [r["y"] for r in res.results]


def _assemble(ys):
    out = np.zeros((2, 4096, D), np.float32)
    for core in range(NCORES):
        b, j = core // 4, core % 4
        out[b, 1024 * j:1024 * (j + 1), :] = ys[core].T
    return out


PLAN = ["ABCSE"]


def kernel(**inputs):
    inp = {k: np.asarray(v) for k, v in inputs.items()}
    ys = None
    for ph in PLAN:
        ys = _run(ph, inp, ys)
    return _assemble(ys)
```
